# Optimizing a Trainium2 kernel written in Bass

```python
import math
import jax
import jax.numpy as jnp
from jax import lax
import numpy as np


D_MODEL = 1024
BATCH = 4
SEQ = 4096
DEPTH = 2

GRID_W = 64
CTX_LEN = 256
HEAD_DIM = 64
N_GROUP_HEADS = 4
GROUP_WIDTH = N_GROUP_HEADS * HEAD_DIM
N_MIXERS = 4
MIX_WIDTH = N_MIXERS * GROUP_WIDTH
A_K_OFF = 1 * GROUP_WIDTH
A_V_OFF = 2 * GROUP_WIDTH
POOL_OFF = 3 * GROUP_WIDTH
SGU_OFF = 4 * GROUP_WIDTH
N_Q_OFF = 6 * GROUP_WIDTH
N_K_OFF = 7 * GROUP_WIDTH
N_V_OFF = 8 * GROUP_WIDTH
PROJ_WIDTH = 9 * GROUP_WIDTH
PROJ_SPLITS = [A_K_OFF, A_V_OFF, POOL_OFF, SGU_OFF, N_Q_OFF, N_K_OFF, N_V_OFF]
DA_QK = HEAD_DIM // 2
ROPE_BASE = 10000.0
Q_BLOCK = 128
POOL_WINDOWS = (2, 4, 8, 16)
POOL_CH = GROUP_WIDTH // len(POOL_WINDOWS)
SGU_CHUNK = 128
WIN_R = 8
WIN_C = 16
NA_QCOLS = 16
NA_SPAN = 2 * WIN_C
N_EXPERTS = 32
TOP_K = 4
D_EXPERT = D_MODEL
SWIGLU_LIMIT = 7.0
SWIGLU_ALPHA = 1.702
MOE_BLOCK = 256
EPS = 1e-6
NEG_INF = -1e30

kernel_name = 'hybrid_parallel_heads_diffusion_block'


def rms_norm(x, g):
    xf = x.astype(jnp.float32)
    y = xf * lax.rsqrt(jnp.mean(xf * xf, axis=-1, keepdims=True) + EPS)
    return (y * g.astype(jnp.float32)).astype(x.dtype)


def _heads(t):
    return None if t is None else t.reshape(t.shape[:-1] + (N_GROUP_HEADS, HEAD_DIM))


def rope_1d(x, pos):
    n = x.shape[-1] // 2
    inv = ROPE_BASE ** (-jnp.arange(n, dtype=jnp.float32) / n)
    ang = pos.astype(jnp.float32)[:, None] * inv[None, :]
    cos = jnp.cos(ang)[None, :, None, :].astype(x.dtype)
    sin = jnp.sin(ang)[None, :, None, :].astype(x.dtype)
    x1, x2 = x[..., :n], x[..., n:]
    return jnp.concatenate([x1 * cos - x2 * sin, x1 * sin + x2 * cos], axis=-1)


def axial_rope(x, row_pos, col_pos):
    h = x.shape[-1] // 2
    return jnp.concatenate([rope_1d(x[..., :h], row_pos), rope_1d(x[..., h:], col_pos)], axis=-1)


def diff_attention(q, k, v, q_ctx, k_ctx, v_ctx, lam_q1, lam_k1, lam_q2, lam_k2, g_sub,
                   lam_init, row_pos, col_pos):
    B, S, H, Dv = v.shape
    lam = (jnp.exp(jnp.sum(lam_q1.astype(jnp.float32) * lam_k1.astype(jnp.float32)))
           - jnp.exp(jnp.sum(lam_q2.astype(jnp.float32) * lam_k2.astype(jnp.float32)))
           + lam_init)
    scale = DA_QK ** -0.5

    def two_maps(qq, kk, vv):
        s1 = jnp.einsum('bqhd,bkhd->bhqk', qq[..., :DA_QK], kk[..., :DA_QK]).astype(jnp.float32) * scale
        s2 = jnp.einsum('bqhd,bkhd->bhqk', qq[..., DA_QK:], kk[..., DA_QK:]).astype(jnp.float32) * scale
        a = jax.nn.softmax(s1, axis=-1) - lam * jax.nn.softmax(s2, axis=-1)
        o = jnp.einsum('bhqk,bkhd->bqhd', a, vv)
        return (rms_norm(o, g_sub) * (1.0 - lam_init)).astype(vv.dtype)

    def rot(t):
        return jnp.concatenate([axial_rope(t[..., :DA_QK], row_pos, col_pos),
                                axial_rope(t[..., DA_QK:], row_pos, col_pos)], axis=-1)

    k_all = jnp.concatenate([rot(k), k_ctx], axis=1)
    v_all = jnp.concatenate([v, v_ctx], axis=1)
    q_blocks = jnp.moveaxis(rot(q).reshape(B, S // Q_BLOCK, Q_BLOCK, H, 2 * DA_QK), 1, 0)
    o = lax.map(lambda qb: two_maps(qb, k_all, v_all), q_blocks)
    o = jnp.moveaxis(o, 0, 1).reshape(B, S, H * Dv)
    o_ctx = None if q_ctx is None else two_maps(q_ctx, k_ctx, v_ctx).reshape(B, -1, H * Dv)
    return o, o_ctx


def ctx_attention(q, k, v):
    s = jnp.einsum('bqhd,bkhd->bhqk', q, k).astype(jnp.float32) * q.shape[-1] ** -0.5
    o = jnp.einsum('bhqk,bkhd->bqhd', jax.nn.softmax(s, axis=-1), v).astype(q.dtype)
    return o.reshape(q.shape[0], q.shape[1], -1)


def neighbourhood_attention(q, k, v, q_ctx, k_ctx, v_ctx, rpb):
    B, S, H, Dh = q.shape
    rows = S // GRID_W
    kr = min(WIN_R, rows)
    ncb = GRID_W // NA_QCOLS
    scale = Dh ** -0.5
    r = jnp.arange(rows)
    blk = jnp.arange(ncb)
    row_idx = jnp.clip(r - kr // 2, 0, rows - kr)[:, None] + jnp.arange(kr)[None, :]
    span_idx = (jnp.clip(blk * NA_QCOLS - WIN_C // 2, 0, GRID_W - NA_SPAN)[:, None]
                + jnp.arange(NA_SPAN)[None, :])
    qcol = blk[:, None] * NA_QCOLS + jnp.arange(NA_QCOLS)[None, :]
    win_start = jnp.clip(qcol - WIN_C // 2, 0, GRID_W - WIN_C)
    kc = span_idx[:, None, :]
    col_ok = (kc >= win_start[..., None]) & (kc < win_start[..., None] + WIN_C)
    mask = jnp.broadcast_to(col_ok[:, :, None, :], (ncb, NA_QCOLS, kr, NA_SPAN))
    mask = mask.reshape(ncb, NA_QCOLS, kr * NA_SPAN)
    rel_r = row_idx - r[:, None] + (WIN_R - 1)
    rel_c = jnp.clip(kc - qcol[..., None], -(WIN_C - 1), WIN_C - 1) + (WIN_C - 1)
    bias = rpb[:, rel_r[:, None, None, :, None], rel_c[None, :, :, None, :]]
    bias = jnp.moveaxis(bias, 0, 2).reshape(rows, ncb, H, NA_QCOLS, kr * NA_SPAN)

    def gather(t):
        tg = t.reshape(B, rows, GRID_W, H, Dh)[:, row_idx[:, None, :, None], span_idx[None, :, None, :]]
        return tg.reshape(B, rows, ncb, kr * NA_SPAN, H, Dh)

    kg, vg = gather(k), gather(v)
    qg = q.reshape(B, rows, ncb, NA_QCOLS, H, Dh)
    s_n = jnp.einsum('brnqhd,brnkhd->brnhqk', qg, kg).astype(jnp.float32) * scale + bias[None]
    s_n = jnp.where(mask[None, None, :, None], s_n, NEG_INF)
    s_c = jnp.einsum('brnqhd,bchd->brnhqc', qg, k_ctx).astype(jnp.float32) * scale
    p = jax.nn.softmax(jnp.concatenate([s_n, s_c], axis=-1), axis=-1)
    nk = kr * NA_SPAN
    o = (jnp.einsum('brnhqk,brnkhd->brnqhd', p[..., :nk], vg)
         + jnp.einsum('brnhqc,bchd->brnqhd', p[..., nk:], v_ctx))
    o = o.astype(q.dtype).reshape(B, S, H * Dh)
    o_ctx = None if q_ctx is None else ctx_attention(q_ctx, k_ctx, v_ctx)
    return o, o_ctx


def pool_mixer(y, w_pool, s_pool):
    L = y.shape[1]
    t = jnp.arange(L)
    yf = y.astype(jnp.float32)
    csum = jnp.concatenate([jnp.zeros_like(yf[:, :1]), jnp.cumsum(yf, axis=1)], axis=1)
    outs = []
    for g, w in enumerate(POOL_WINDOWS):
        sl = slice(g * POOL_CH, (g + 1) * POOL_CH)
        lo = jnp.clip(t - w // 2, 0, L - 1)
        hi = jnp.clip(t + (w - w // 2 - 1), 0, L - 1)
        cnt = (hi - lo + 1).astype(jnp.float32)[None, :, None]
        mean = (csum[:, hi + 1, sl] - csum[:, lo, sl]) / cnt
        outs.append((mean - yf[..., sl]).astype(y.dtype) @ w_pool[g])
    return jnp.concatenate(outs, axis=-1) * s_pool


def sgu_mixer(z, g_sgu, w_sgu, b_sgu):
    B, L, _ = z.shape
    z = jax.nn.gelu(z)
    u, v = z[..., :GROUP_WIDTH], z[..., GROUP_WIDTH:]
    v = rms_norm(v, g_sgu).reshape(B, L // SGU_CHUNK, SGU_CHUNK, N_GROUP_HEADS, HEAD_DIM)
    s = jnp.einsum('gts,bnsgc->bntgc', w_sgu, v) + b_sgu.T[:, :, None]
    return u * s.reshape(B, L, GROUP_WIDTH)


def moe_ffn(h, w_router, b_router, w_gu, b_gu, w_down, b_down):
    T, D = h.shape
    logits = h.astype(jnp.float32) @ w_router.astype(jnp.float32) + b_router.astype(jnp.float32)
    top_val, top_idx = lax.top_k(logits, TOP_K)
    gates = jax.nn.softmax(top_val, axis=-1)
    M = T * TOP_K
    flat_e = top_idx.reshape(-1)
    order = jnp.argsort(flat_e)
    sorted_e = flat_e[order]
    counts = jnp.bincount(flat_e, length=N_EXPERTS)
    padded = (counts + MOE_BLOCK - 1) // MOE_BLOCK * MOE_BLOCK
    start = jnp.cumsum(counts) - counts
    cum_pad = jnp.cumsum(padded)
    pstart = cum_pad - padded
    dest = pstart[sorted_e] + jnp.arange(M) - start[sorted_e]
    nb = -(-M // MOE_BLOCK) + N_EXPERTS
    slot_tok = jnp.zeros((nb * MOE_BLOCK,), jnp.int32).at[dest].set((order // TOP_K).astype(jnp.int32))
    blk_e = jnp.minimum(jnp.searchsorted(cum_pad, jnp.arange(nb) * MOE_BLOCK, side='right'), N_EXPERTS - 1)

    def expert_block(args):
        tok, e = args
        xb = h[tok]
        gu = xb @ w_gu[e] + b_gu[e]
        gt = jnp.minimum(gu[..., :D_EXPERT], SWIGLU_LIMIT)
        up = jnp.clip(gu[..., D_EXPERT:], -SWIGLU_LIMIT, SWIGLU_LIMIT)
        act = (up + 1.0) * gt * jax.nn.sigmoid(SWIGLU_ALPHA * gt)
        return act @ w_down[e] + b_down[e]

    yb = lax.map(expert_block, (slot_tok.reshape(nb, MOE_BLOCK), blk_e)).reshape(nb * MOE_BLOCK, D)
    y = jnp.zeros((M, D), yb.dtype).at[order].set(yb[dest]).reshape(T, TOP_K, D)
    return jnp.einsum('tkd,tk->td', y, gates.astype(y.dtype))


def hybrid_layer(x, xc, c, c_ctx, p, layer_idx, update_ctx):
    B, S, D = x.shape
    t = jnp.arange(S)
    row_pos, col_pos = t // GRID_W, t % GRID_W
    lam_init = 0.8 - 0.6 * math.exp(-0.3 * layer_idx)
    mod = (jax.nn.silu(c) @ p['w_mod'] + p['b_mod'])[:, None, :]
    sh1, sc1, gt1, sh2, sc2, gt2 = jnp.split(mod, 6, axis=-1)
    n_cm = 6 if update_ctx else 2
    modc = jnp.split(jax.nn.silu(c_ctx) @ p['w_mod'][:, :n_cm * D] + p['b_mod'][:n_cm * D], n_cm, axis=-1)

    h = rms_norm(x, p['g_mix']) * (1 + sc1) + sh1
    hc = rms_norm(xc, p['g_mix']) * (1 + modc[1]) + modc[0]
    aq, ak, av, pool_in, sgu_in, nq, nk, nv = jnp.split(h @ p['w_in'], PROJ_SPLITS, axis=-1)
    if update_ctx:
        aqc, akc, avc, pool_c, sgu_c, nqc, nkc, nvc = jnp.split(hc @ p['w_in'], PROJ_SPLITS, axis=-1)
    else:
        akc, avc = jnp.split(hc @ p['w_in'][:, A_K_OFF:POOL_OFF], 2, axis=-1)
        nkc, nvc = jnp.split(hc @ p['w_in'][:, N_K_OFF:], 2, axis=-1)
        aqc = nqc = None
    oa, oa_c = diff_attention(_heads(aq), _heads(ak), _heads(av), _heads(aqc), _heads(akc), _heads(avc),
                              p['lam_q1'], p['lam_k1'], p['lam_q2'], p['lam_k2'], p['g_sub'],
                              lam_init, row_pos, col_pos)
    od, od_c = neighbourhood_attention(_heads(nq), _heads(nk), _heads(nv), _heads(nqc), _heads(nkc),
                                       _heads(nvc), p['rpb'])
    mix = jnp.concatenate([oa, pool_mixer(pool_in, p['w_pool'], p['s_pool']),
                           sgu_mixer(sgu_in, p['g_sgu'], p['w_sgu'], p['b_sgu']), od], axis=-1)
    x = x + gt1 * (mix @ p['w_out'])

    hf = rms_norm(x, p['g_ffn']) * (1 + sc2) + sh2
    if update_ctx:
        mixc = jnp.concatenate([oa_c, pool_mixer(pool_c, p['w_pool'], p['s_pool']),
                                sgu_mixer(sgu_c, p['g_sgu'], p['w_sgu'], p['b_sgu']), od_c], axis=-1)
        xc = xc + modc[2] * (mixc @ p['w_out'])
        hfc = rms_norm(xc, p['g_ffn']) * (1 + modc[4]) + modc[3]
        y = moe_ffn(jnp.concatenate([hf.reshape(-1, D), hfc.reshape(-1, D)], axis=0), p['w_router'],
                    p['b_router'], p['w_gu'], p['b_gu'], p['w_down'], p['b_down'])
        x = x + gt2 * y[:B * S].reshape(B, S, D)
        xc = xc + modc[5] * y[B * S:].reshape(xc.shape)
    else:
        y = moe_ffn(hf.reshape(-1, D), p['w_router'], p['b_router'], p['w_gu'], p['b_gu'],
                    p['w_down'], p['b_down'])
        x = x + gt2 * y.reshape(B, S, D)
    return x, xc


def setup_inputs(seed: int = 0) -> dict:
    key = jax.random.key(seed)
    ks = jax.random.split(key, 32)
    f32 = jnp.float32

    def nrm(i, shape, s):
        return jax.random.normal(ks[i], shape, f32) * s

    L, D, G, E, F = DEPTH, D_MODEL, GROUP_WIDTH, N_EXPERTS, D_EXPERT
    return {
        'x': nrm(0, (BATCH, SEQ, D), 1.0),
        'c': nrm(1, (BATCH, D), 1.0),
        'ctx': nrm(2, (BATCH, CTX_LEN, D), 1.0),
        'c_ctx': nrm(3, (D,), 1.0),
        'w_mod': nrm(4, (L, D, 6 * D), 0.5 * D ** -0.5),
        'b_mod': nrm(5, (L, 6 * D), 0.02),
        'g_mix': 1.0 + nrm(6, (L, D), 0.05),
        'g_ffn': 1.0 + nrm(7, (L, D), 0.05),
        'w_in': nrm(8, (L, D, PROJ_WIDTH), D ** -0.5),
        'w_out': nrm(9, (L, MIX_WIDTH, D), MIX_WIDTH ** -0.5),
        'lam_q1': nrm(10, (L, DA_QK), 0.1),
        'lam_k1': nrm(11, (L, DA_QK), 0.1),
        'lam_q2': nrm(12, (L, DA_QK), 0.1),
        'lam_k2': nrm(13, (L, DA_QK), 0.1),
        'g_sub': 1.0 + nrm(14, (L, HEAD_DIM), 0.05),
        'w_pool': nrm(15, (L, len(POOL_WINDOWS), POOL_CH, POOL_CH), POOL_CH ** -0.5),
        's_pool': 1.0 + nrm(16, (L, G), 0.1),
        'g_sgu': 1.0 + nrm(17, (L, G), 0.05),
        'w_sgu': nrm(18, (L, N_GROUP_HEADS, SGU_CHUNK, SGU_CHUNK), SGU_CHUNK ** -0.5),
        'b_sgu': 1.0 + nrm(19, (L, N_GROUP_HEADS, SGU_CHUNK), 0.1),
        'rpb': nrm(20, (L, N_GROUP_HEADS, 2 * WIN_R - 1, 2 * WIN_C - 1), 0.1),
        'w_router': nrm(21, (L, D, E), D ** -0.5),
        'b_router': nrm(22, (L, E), 0.01),
        'w_gu': nrm(23, (L, E, D, 2 * F), D ** -0.5),
        'b_gu': nrm(24, (L, E, 2 * F), 0.01),
        'w_down': nrm(25, (L, E, F, D), F ** -0.5),
        'b_down': nrm(26, (L, E, D), 0.01),
        'g_final': 1.0 + nrm(27, (D,), 0.05),
    }


def reference(x, c, ctx, c_ctx, w_mod, b_mod, g_mix, g_ffn, w_in, w_out, lam_q1, lam_k1, lam_q2,
              lam_k2, g_sub, w_pool, s_pool, g_sgu, w_sgu, b_sgu, rpb, w_router, b_router, w_gu,
              b_gu, w_down, b_down, g_final):
    xc = ctx
    for l in range(DEPTH):
        p = dict(w_mod=w_mod[l], b_mod=b_mod[l], g_mix=g_mix[l], g_ffn=g_ffn[l], w_in=w_in[l],
                 w_out=w_out[l], lam_q1=lam_q1[l], lam_k1=lam_k1[l], lam_q2=lam_q2[l],
                 lam_k2=lam_k2[l], g_sub=g_sub[l], w_pool=w_pool[l], s_pool=s_pool[l],
                 g_sgu=g_sgu[l], w_sgu=w_sgu[l], b_sgu=b_sgu[l], rpb=rpb[l],
                 w_router=w_router[l], b_router=b_router[l], w_gu=w_gu[l], b_gu=b_gu[l],
                 w_down=w_down[l], b_down=b_down[l])
        x, xc = hybrid_layer(x, xc, c, c_ctx, p, l, l < DEPTH - 1)
    return rms_norm(x, g_final)
```

```python
import math
import numpy as np

D = 1024
GRID = 64
HALF_ROWS = 32
T_OWN = 2048
CTX = 256
NEG = -1e30
LAM_INIT = [0.8 - 0.6 * math.exp(-0.3 * l) for l in range(2)]


def own_token_idx(par):
    rows = np.arange(HALF_ROWS) if par == 0 else 63 - np.arange(HALF_ROWS)
    return (rows[:, None] * GRID + np.arange(GRID)[None, :]).reshape(-1)


G_IDX = np.concatenate([own_token_idx(0), own_token_idx(1)])


def rope_table(tok_idx, n_ctx):
    inv = 10000.0 ** (-np.arange(8, dtype=np.float32) / 8.0)
    row = (tok_idx // GRID).astype(np.float32)
    col = (tok_idx % GRID).astype(np.float32)
    n = tok_idx.shape[0]
    cos = np.zeros((32, n + n_ctx), np.float32)
    sin = np.zeros((32, n + n_ctx), np.float32)
    for d in range(32):
        pos = row if d < 16 else col
        dd = d % 16
        j = dd % 8
        ang = pos * inv[j]
        cos[d, :n] = np.cos(ang)
        sin[d, :n] = np.sin(ang) * (-1.0 if dd < 8 else 1.0)
    cos[:, n:] = 1.0
    sin[:, n:] = 0.0
    return np.stack([np.tile(cos, (4, 1)), np.tile(sin, (4, 1))]).astype(np.float32)


def rh_perm():
    p = np.arange(256)
    d = p % 16
    return np.where(d < 8, p + 8, p - 8)


def w_all_cols():
    G = 256
    aq = np.arange(0, G); ak = np.arange(G, 2 * G); av = np.arange(2 * G, 3 * G)
    pool = np.arange(3 * G, 4 * G); sgu = np.arange(4 * G, 6 * G)
    nq = np.arange(6 * G, 7 * G); nk = np.arange(7 * G, 8 * G); nv = np.arange(8 * G, 9 * G)
    rh = rh_perm()
    return np.concatenate([aq, aq[rh], ak, ak[rh], av, pool, sgu, nq, nk, nv])


C_AQ, C_AQR, C_AK, C_AKR, C_AV, C_POOL, C_SGU, C_NQ, C_NK, C_NV = 0, 256, 512, 768, 1024, 1280, 1536, 2048, 2304, 2560
NCOL = 2816


def pool_full_matrix(L):
    t = np.arange(L)
    outs = []
    for w in (2, 4, 8, 16):
        lo = np.clip(t - w // 2, 0, L - 1)
        hi = np.clip(t + (w - w // 2 - 1), 0, L - 1)
        cnt = (hi - lo + 1).astype(np.float32)
        M = np.zeros((L, L), np.float32)
        for i in range(L):
            M[i, lo[i]:hi[i] + 1] = 1.0 / cnt[i]
        M -= np.eye(L, dtype=np.float32)
        outs.append(M)
    return outs


_POOL_LAT = None
_POOL_CTX = None


def pool_band_tables(par):
    global _POOL_LAT, _POOL_CTX
    if _POOL_LAT is None:
        _POOL_LAT = pool_full_matrix(4096)
        _POOL_CTX = pool_full_matrix(256)
    own = own_token_idx(par)
    oth = own_token_idx(1 - par)
    out = np.zeros((128, 52, 128), np.float32)

    def blk(M, tgt, src):
        return M[np.ix_(tgt, src)].T

    ch = lambda idx, c: idx[c * 128:(c + 1) * 128]
    for g in range(4):
        M = _POOL_LAT[g]
        b = 9 * g
        out[:, b + 0] = blk(M, ch(own, 0), ch(own, 0))
        out[:, b + 1] = blk(M, ch(own, 0), ch(own, 1))
        out[:, b + 2] = blk(M, ch(own, 5), ch(own, 4))
        out[:, b + 3] = blk(M, ch(own, 5), ch(own, 5))
        out[:, b + 4] = blk(M, ch(own, 5), ch(own, 6))
        out[:, b + 5] = blk(M, ch(own, 15), ch(own, 14))
        out[:, b + 6] = blk(M, ch(own, 15), ch(own, 15))
        cand = [ch(own_token_idx(0), 15), ch(own_token_idx(1), 15)]
        for s in range(2):
            if s != par:
                out[:, b + 7 + s] = blk(M, ch(own, 15), cand[s])
        Mc = _POOL_CTX[g]
        c0 = np.arange(0, 128); c1 = np.arange(128, 256)
        b = 36 + 4 * g
        out[:, b + 0] = blk(Mc, c0, c0)
        out[:, b + 1] = blk(Mc, c0, c1)
        out[:, b + 2] = blk(Mc, c1, c0)
        out[:, b + 3] = blk(Mc, c1, c1)
    return out


def sgu_perm(par):
    p = np.arange(128)
    if par == 0:
        return p
    lr = p // 64
    c = p % 64
    return (1 - lr) * 64 + c


def sgu_tables(w_sgu, b_sgu, par):
    wsT = np.zeros((128, 2, 4, 128), np.float32)
    bfull = np.zeros((128, 2, 256), np.float32)
    for v, perm in enumerate((sgu_perm(par), np.arange(128))):
        for g in range(4):
            Wp = w_sgu[g][np.ix_(perm, perm)]
            wsT[:, v, g, :] = Wp.T
            bfull[:, v, g * 64:(g + 1) * 64] = b_sgu[g][perm][:, None]
    return wsT, bfull


def nbr_variant(rl):
    return rl if rl < 4 else (4 if rl <= 26 else rl - 22)


def nbr_rows_for_variant(v):
    return v if v < 4 else (10 if v == 4 else v + 22)


def nbr_astart(rl):
    return int(np.clip(rl - 4, 0, 22))


def nbr_tables(rpb, par):
    out = np.full((10, 64, 4, 9, 128), NEG, np.float32)
    qc = np.arange(64)
    kc = np.arange(64)
    win_start = np.clip(qc - 8, 0, 48)
    col_ok = (kc[None, :] >= win_start[:, None]) & (kc[None, :] < win_start[:, None] + 16)
    rel_c = np.clip(kc[None, :] - qc[:, None], -15, 15) + 15
    g_of = lambda lr, p: lr if p == 0 else 63 - lr
    for v in range(10):
        rl = nbr_rows_for_variant(v)
        gq = g_of(rl, par)
        lo = int(np.clip(gq - 4, 0, 56))
        a = nbr_astart(rl)
        slots = []
        for m in range(5):
            slots.append([g_of(a + 2 * m, par), g_of(a + 2 * m + 1, par)])
        for s in range(2):
            for r0 in (28, 30):
                if s != par:
                    slots.append([g_of(r0, s), g_of(r0 + 1, s)])
                else:
                    slots.append([None, None])
        for si, rows in enumerate(slots):
            for half, gk in enumerate(rows):
                if gk is None or not (lo <= gk < lo + 8):
                    continue
                rel_r = gk - gq + 7
                for h in range(4):
                    vals = rpb[h, rel_r][rel_c]
                    out[v, :, h, si, half * 64:(half + 1) * 64] = np.where(col_ok, vals, NEG)
    return out


from contextlib import ExitStack
import numpy as np
import concourse.bass as bass
import concourse.mybir as mybir
from concourse.bass_utils import run_bass_kernel_spmd

F32 = mybir.dt.float32
BF16 = mybir.dt.bfloat16
I32 = mybir.dt.int32
AF = mybir.ActivationFunctionType
ALU = mybir.AluOpType
AX = mybir.AxisListType
EPS = 1e-6
NB_BLK = 68
BLK = 256


class Tk:
    __slots__ = ("name", "w", "r", "dsem", "dcnt")

    def __init__(self, name):
        self.name = name
        self.w = None
        self.r = {}
        self.dsem = None
        self.dcnt = 0


class Eng:
    def __init__(self, fw, name, eng):
        self.name = name
        self.eng = eng
        self.sem = fw.new_sem("s_" + name)
        self.cnt = 0
        self.seen = {}


class FW:
    def __init__(self, nc, stack, sfx=""):
        self.nc = nc
        self.stack = stack
        self.sfx = sfx
        self.nsem = 0
        self.E = {}
        self.dma_toks = []
        self.pool = []
        for nm, e in (("pe", nc.tensor), ("act", nc.scalar), ("dve", nc.vector),
                      ("pool", nc.gpsimd), ("sp", nc.sync)):
            self.E[nm] = Eng(self, nm, e)
        self.ninst = 0

    def new_sem(self, name):
        self.nsem += 1
        return self.stack.enter_context(self.nc.semaphore("%s%s_%d" % (name, self.sfx, self.nsem)))

    def _need(self, E, rec):
        if rec is None:
            return
        kind, s, c = rec
        if kind == 'e':
            if s == E.name and (s == "pe" or c > E.cnt):
                return
            sem = self.E[s].sem
        else:
            sem = s
        key = id(sem)
        if E.seen.get(key, 0) >= c:
            return
        E.eng.wait_ge(sem, c)
        E.seen[key] = c

    def deps(self, E, reads, writes):
        for t in reads:
            self._need(E, t.w)
        for t in writes:
            self._need(E, t.w)
            for rec in t.r.values():
                self._need(E, rec)

    def op(self, en, fn, reads=(), writes=(), inc=True):
        E = self.E[en]
        self.deps(E, reads, writes)
        ins = fn(E.eng)
        self.ninst += 1
        c = E.cnt + 1
        if inc:
            ins.then_inc(E.sem, 1)
            E.cnt = c
        rec = ('e', en, c)
        for t in reads:
            t.r[en] = rec
        for t in writes:
            t.w = rec
            t.r = {}
        return ins

    def _dma_done(self, ins, reads, writes, multi):
        tk = writes[0] if writes else reads[0]
        if tk.dsem is None:
            if self.pool:
                tk.dsem, tk.dcnt = self.pool.pop()
            else:
                tk.dsem = self.new_sem("d_" + tk.name)
            self.dma_toks.append(tk)
        tk.dcnt += 16
        ins.then_inc(tk.dsem, 16)
        rec = ('d', tk.dsem, tk.dcnt)
        for t in reads:
            t.r['dma' + str(id(tk))] = rec
        for t in writes:
            t.w = rec
            if not multi:
                t.r = {}
        self.ninst += 1
        return ins

    def dma(self, en, out_ap, in_ap, reads=(), writes=(), multi=False, **kw):
        E = self.E[en]
        if multi:
            for t in reads:
                self._need(E, t.w)
        else:
            self.deps(E, reads, writes)
        ins = E.eng.dma_start(out=out_ap, in_=in_ap, **kw)
        return self._dma_done(ins, reads, writes, multi)

    def idma(self, out_ap, out_off, in_ap, in_off, reads=(), writes=(), multi=False, **kw):
        E = self.E["pool"]
        if multi:
            for t in reads:
                self._need(E, t.w)
        else:
            self.deps(E, reads, writes)
        ins = E.eng.indirect_dma_start(out=out_ap, out_offset=out_off, in_=in_ap, in_offset=in_off, **kw)
        return self._dma_done(ins, reads, writes, multi)

    def wait_all(self, en, toks):
        E = self.E[en]
        for t in toks:
            self._need(E, t.w)
            for rec in t.r.values():
                self._need(E, rec)

    def recycle(self):
        self.barrier()
        for t in self.dma_toks:
            self.pool.append((t.dsem, t.dcnt))
        self.dma_toks = []

    def barrier(self):
        for E in self.E.values():
            for E2 in self.E.values():
                if E2 is not E and E2.cnt > 0:
                    self._need(E, ('e', E2.name, E2.cnt))
            for t in self.dma_toks:
                self._need(E, ('d', t.dsem, t.dcnt))


class _Stop(Exception):
    pass


_DECL = {}
_SHARED = ("ropeK", "ropeQ", "pband", "cT", "g_fin")


def build_layer(l, NCOL, lam_init, dbg=False, stop=None, nc=None, io=None, sfx=""):
    upd = (l == 0)
    last = (l == 1)
    NTT = 18 if upd else 16
    if nc is None:
        nc = bass.Bass("TRN2", target_bir_lowering=False)
    io = io or {}
    NB = 68 if upd else 64

    def dt_in(name, shape, dt=F32):
        full = name if name in _SHARED else name + sfx
        key = (id(nc), full)
        if key not in _DECL:
            _DECL[key] = nc.dram_tensor(full, shape, dt, kind="ExternalInput").ap()
        return _DECL[key]
    x_own = io["x_own"] if "x_own" in io else dt_in("x_own", [2048, 1024])
    Gx = io["G"] if "G" in io else dt_in("G", [4096, 1024])
    xc = io["xc"] if "xc" in io else dt_in("xc", [256, 1024])
    cT = dt_in("cT", [128, 16])
    w_mod = dt_in("w_mod", [1024, 6144])
    b_mod = dt_in("b_mod", [1, 6144])
    g_mix = dt_in("g_mix", [1, 1024])
    g_ffn = dt_in("g_ffn", [1, 1024])
    g_fin = dt_in("g_fin", [1, 1024])
    W_all = dt_in("W_all", [1024, NCOL])
    w_out = dt_in("w_out", [1024, 1024])
    lamv = dt_in("lamv", [1, 128])
    g_sub = dt_in("g_sub", [128, 1])
    w_pool = dt_in("w_pool", [64, 4, 128])
    s_pool = dt_in("s_pool", [128, 2])
    g_sgu = dt_in("g_sgu", [1, 256])
    wsT = dt_in("wsT", [128, 2, 4, 128])
    bfull = dt_in("bfull", [128, 2, 256])
    ntab = dt_in("ntab", [10, 64, 4 * 9 * 128])
    ropeK = dt_in("ropeK", [2, 128, 4352])
    ropeQ = dt_in("ropeQ", [2, 128, 2304])
    pband = dt_in("pband", [128, 52, 128])
    w_router = dt_in("w_router", [1024, 32])
    b_router = dt_in("b_router", [1, 32])
    if stop is None or stop.partition("#")[0] not in ("mod", "proj", "pool", "nbr", "nbrA", "nbrB", "nbrC", "kv", "diff", "xmid", "route"):
        w_gu = dt_in("w_gu", [32 * 128, 8 * 2048])
        b_gu = dt_in("b_gu", [32, 2048])
        w_down = dt_in("w_down", [32 * 128, 8 * 1024])
        b_down = dt_in("b_down", [32, 1024])
    x_new = io["x_new"] if "x_new" in io else nc.dram_tensor("x_new" + sfx, [2048, 1024], F32, kind="ExternalOutput").ap()
    xc_new = io["xc_new"] if "xc_new" in io else nc.dram_tensor("xc_new" + sfx, [256, 1024], F32, kind="ExternalOutput").ap()
    modv = nc.dram_tensor("modv" + sfx, [2, 6144], F32, kind="Internal").ap()
    xmid = nc.dram_tensor("xmid" + sfx, [NTT * 128, 1024], F32, kind="Internal").ap()
    xs_d = nc.dram_tensor("xs_d" + sfx, [NB * BLK, 1024], BF16, kind="Internal").ap()
    yb_d = nc.dram_tensor("yb_d" + sfx, [NB * BLK, 1024], F32, kind="Internal").ap()
    dbg_out = {}
    if dbg:
        dbg_out["d_mix"] = nc.dram_tensor("d_mix", [1024, 2304], F32, kind="ExternalOutput").ap()
        dbg_out["d_mod"] = nc.dram_tensor("d_mod", [2, 6144], F32, kind="ExternalOutput").ap()
        dbg_out["d_xmid"] = nc.dram_tensor("d_xmid", [NTT * 128, 1024], F32, kind="ExternalOutput").ap()
        dbg_out["d_log"] = nc.dram_tensor("d_log", [NTT * 128, 32], F32, kind="ExternalOutput").ap()
        dbg_out["d_dest"] = nc.dram_tensor("d_dest", [NTT * 128, 8], F32, kind="ExternalOutput").ap()
        dbg_out["d_sm"] = nc.dram_tensor("d_sm", [128, 16 * 32], F32, kind="ExternalOutput").ap()
        dbg_out["d_mask"] = nc.dram_tensor("d_mask", [128, NTT * 32], F32, kind="ExternalOutput").ap()
        dbg_out["d_maskb"] = nc.dram_tensor("d_maskb", [128, NTT * 32], F32, kind="ExternalOutput").ap()
        dbg_out["d_dm"] = nc.dram_tensor("d_dm", [NTT * 128, 32], F32, kind="ExternalOutput").ap()

    try:
      with ExitStack() as top:
        fw = io["fw"] if "fw" in io else FW(nc, top, sfx)
        _chkc = {}
        def chk(name):
            if stop is None:
                return
            base, _, nth = stop.partition("#")
            if base == name:
                _chkc[name] = _chkc.get(name, 0) + 1
                if _chkc[name] >= int(nth or 1):
                    fw.barrier()
                    raise _Stop()
        op, dma = fw.op, fw.dma
        t_modv, t_xmid, t_xs, t_yb, t_xnew, t_xcnew = (Tk(n) for n in ("modv", "xmid", "xs", "yb", "xnew", "xcnew"))

        def alloc(stack, name, shape, dt, psum=False):
            name = name + sfx
            if psum:
                return stack.enter_context(nc.psum_tensor(name, shape, dt))
            return stack.enter_context(nc.sbuf_tensor(name, shape, dt))

        ident_f = alloc(top, "ident_f", [128, 128], F32); t_c = Tk("consts")
        ident_b = alloc(top, "ident_b", [128, 128], BF16)
        ones_f = alloc(top, "ones_f", [128, 128], F32)
        ones_b = alloc(top, "ones_b", [128, 512], BF16)
        od64 = alloc(top, "od64", [128, 64], F32)
        op("pool", lambda e: e.memset(ident_f[:], 0.0), writes=[t_c])
        op("pool", lambda e: e.affine_select(out=ident_f[:], in_=ident_f[:], pattern=[[-1, 128]],
                                             compare_op=ALU.not_equal, fill=1.0, base=0, channel_multiplier=1),
           reads=[t_c], writes=[t_c])
        op("dve", lambda e: e.tensor_copy(out=ident_b[:], in_=ident_f[:]), reads=[t_c], writes=[t_c])
        op("dve", lambda e: e.memset(ones_f[:], 1.0), writes=[t_c])
        op("dve", lambda e: e.memset(ones_b[:], 1.0), writes=[t_c])
        op("dve", lambda e: e.memset(od64[:], 1.0 / 64.0), writes=[t_c])
        cm05 = alloc(top, "cm05", [128, 512], F32)
        op("dve", lambda e: e.memset(cm05[:], -0.5), writes=[t_c])

        if io.get("pre_barrier"):
            fw.barrier()
        with ExitStack() as ph:
            sc = alloc(ph, "sc", [128, 16], F32); t_sc = Tk("sc")
            dma("sp", sc[:], cT, writes=[t_sc])
            op("act", lambda e: e.activation(out=sc[:], in_=sc[:], func=AF.Silu), reads=[t_sc], writes=[t_sc])
            wm = [alloc(ph, "wm%d" % i, [128, 8, 512], F32) for i in range(2)]
            t_wm = [Tk("wm%d" % i) for i in range(2)]
            pm = [alloc(ph, "pm%d" % i, [128, 512], F32, psum=True) for i in range(2)]
            t_pm = [Tk("pm%d" % i) for i in range(2)]
            raw = alloc(ph, "mraw", [2, 6144], F32); t_raw = Tk("mraw")
            fin = alloc(ph, "mfin", [2, 6144], F32); t_fin = Tk("mfin")
            bm = alloc(ph, "bm", [2, 6144], F32); t_bm = Tk("bm")
            gm2 = alloc(ph, "gm2", [2, 2048], F32); t_gm2 = Tk("gm2")
            dma("sp", bm[:], b_mod.to_broadcast([2, 6144]), writes=[t_bm])
            dma("sp", gm2[:, 0:1024], g_mix.to_broadcast([2, 1024]), writes=[t_gm2])
            dma("sp", gm2[:, 1024:2048], g_ffn.to_broadcast([2, 1024]), writes=[t_gm2], multi=True)
            wmv = w_mod.rearrange("(k p) c -> p k c", p=128)
            for cb in range(12):
                b = cb % 2
                dma("sp", wm[b][:], wmv[:, :, cb * 512:(cb + 1) * 512], writes=[t_wm[b]])
                for k in range(8):
                    op("pe", lambda e, k=k, b=b: e.matmul(pm[b][0:2, :], lhsT=sc[:, 2 * k:2 * k + 2], rhs=wm[b][:, k, :],
                                                          start=(k == 0), stop=(k == 7)),
                       reads=[t_sc, t_wm[b]], writes=[t_pm[b]], inc=(k == 7))
                op("dve", lambda e, b=b, cb=cb: e.tensor_tensor(out=raw[:, cb * 512:(cb + 1) * 512], in0=pm[b][0:2, :],
                                                                in1=bm[:, cb * 512:(cb + 1) * 512], op=ALU.add),
                   reads=[t_pm[b], t_bm], writes=[t_raw])
            S = lambda i: slice(i * 1024, (i + 1) * 1024)
            op("dve", lambda e: e.scalar_tensor_tensor(out=fin[:, S(0)], in0=raw[:, S(1)], scalar=1.0, in1=gm2[:, 0:1024],
                                                       op0=ALU.add, op1=ALU.mult), reads=[t_raw, t_gm2], writes=[t_fin])
            op("dve", lambda e: e.tensor_copy(out=fin[:, S(1)], in_=raw[:, S(0)]), reads=[t_raw], writes=[t_fin])
            op("dve", lambda e: e.tensor_copy(out=fin[:, S(2)], in_=raw[:, S(2)]), reads=[t_raw], writes=[t_fin])
            op("dve", lambda e: e.scalar_tensor_tensor(out=fin[:, S(3)], in0=raw[:, S(4)], scalar=1.0, in1=gm2[:, 1024:2048],
                                                       op0=ALU.add, op1=ALU.mult), reads=[t_raw, t_gm2], writes=[t_fin])
            op("dve", lambda e: e.tensor_copy(out=fin[:, S(4)], in_=raw[:, S(3)]), reads=[t_raw], writes=[t_fin])
            op("dve", lambda e: e.tensor_copy(out=fin[:, S(5)], in_=raw[:, S(5)]), reads=[t_raw], writes=[t_fin])
            dma("sp", modv, fin[:], reads=[t_fin], writes=[t_modv])
            chk("mod")
            if dbg:
                dma("sp", dbg_out["d_mod"], fin[:], reads=[t_fin])
            fw.barrier()

        def load_mod(tile, tk, row, seg):
            dma("sp", tile[:], modv[row:row + 1, seg * 1024:(seg + 1) * 1024].to_broadcast([128, 1024]),
                reads=[t_modv], writes=[tk])

        with ExitStack() as mixph:
            mixT = alloc(mixph, "mixT", [128, 8, 2304], BF16)
            t_mix = [Tk("mix%d" % i) for i in range(8)]
            qA = alloc(mixph, "qA", [128, 2, 2304], BF16); qB = alloc(mixph, "qB", [128, 2, 2304], BF16)
            t_q = Tk("q")
            hcT = alloc(mixph, "hcT", [128, 8, 256], BF16); t_hcT = Tk("hcT")
            mA = alloc(mixph, "mA", [128, 1024], F32); t_mA = Tk("mA")
            mB = alloc(mixph, "mB", [128, 1024], F32); t_mB = Tk("mB")
            xt = [alloc(mixph, "xt%d" % i, [128, 1024], F32) for i in range(2)]
            t_xt = [Tk("xt%d" % i) for i in range(2)]
            tmpf = alloc(mixph, "tmpf", [128, 1024], F32); t_tmpf = Tk("tmpf")
            hb = [alloc(mixph, "hb%d" % i, [128, 1024], BF16) for i in range(2)]
            t_hb = [Tk("hb%d" % i) for i in range(2)]
            ss = alloc(mixph, "ss", [128, 8], F32); t_ss = Tk("ss")
            hT = alloc(mixph, "hT", [128, 8, 512], BF16); t_hT = Tk("hT")
            Wb = alloc(mixph, "Wb", [128, 8, 2048], BF16); t_W = Tk("W")
            maskA = alloc(mixph, "maskA", [128, 2], F32); t_mk = Tk("maskAB")
            nlam = alloc(mixph, "nlam", [128, 4], F32); t_nlam = Tk("nlam")
            gs1 = alloc(mixph, "gs1", [128, 1], F32); t_gs1 = Tk("gs1")
            Wv = W_all.rearrange("(k p) c -> p k c", p=128)
            cnt = {"x": 0, "h": 0}

            op("pool", lambda e: e.memset(maskA[:], 0.0), writes=[t_mk])
            for base in (0, 64):
                op("pool", lambda e, base=base: e.memset(maskA[base:base + 32, 0:1], 1.0), writes=[t_mk])
                op("pool", lambda e, base=base: e.memset(maskA[base + 32:base + 64, 1:2], 1.0), writes=[t_mk])
            with ExitStack() as tmp:
                lv = alloc(tmp, "lv", [128, 128], F32); t_lv = Tk("lv")
                dma("sp", lv[:], lamv.to_broadcast([128, 128]), writes=[t_lv])
                op("dve", lambda e: e.tensor_tensor(out=lv[:, 0:32], in0=lv[:, 0:32], in1=lv[:, 32:64], op=ALU.mult),
                   reads=[t_lv], writes=[t_lv])
                op("dve", lambda e: e.tensor_tensor(out=lv[:, 64:96], in0=lv[:, 64:96], in1=lv[:, 96:128], op=ALU.mult),
                   reads=[t_lv], writes=[t_lv])
                op("dve", lambda e: e.reduce_sum(out=nlam[:, 0:1], in_=lv[:, 0:32], axis=AX.X), reads=[t_lv], writes=[t_nlam])
                op("dve", lambda e: e.reduce_sum(out=nlam[:, 1:2], in_=lv[:, 64:96], axis=AX.X), reads=[t_lv], writes=[t_nlam])
                op("act", lambda e: e.activation(out=nlam[:, 0:2], in_=nlam[:, 0:2], func=AF.Exp), reads=[t_nlam], writes=[t_nlam])
                op("dve", lambda e: e.tensor_tensor(out=nlam[:, 2:3], in0=nlam[:, 1:2], in1=nlam[:, 0:1], op=ALU.subtract),
                   reads=[t_nlam], writes=[t_nlam])
                op("dve", lambda e: e.tensor_scalar(out=nlam[:, 3:4], in0=nlam[:, 2:3], scalar1=-lam_init, scalar2=None, op0=ALU.add),
                   reads=[t_nlam], writes=[t_nlam])
                dma("sp", gs1[:], g_sub, writes=[t_gs1])
                op("dve", lambda e: e.tensor_scalar(out=gs1[:], in0=gs1[:], scalar1=1.0 - lam_init, scalar2=None, op0=ALU.mult),
                   reads=[t_gs1], writes=[t_gs1])
                fw.barrier()

            def norm_to_hT(src_rows, col0, psT, t_psT, dst=None, t_dst=None):
                dst = hT if dst is None else dst
                t_dst = t_hT if t_dst is None else t_dst
                i = cnt["x"] % 2; cnt["x"] += 1
                dma("sp", xt[i][:], src_rows, writes=[t_xt[i]])
                op("act", lambda e: e.activation(out=tmpf[:], in_=xt[i][:], func=AF.Square, accum_out=ss[:, 0:1]),
                   reads=[t_xt[i]], writes=[t_tmpf, t_ss])
                op("dve", lambda e: e.tensor_scalar(out=ss[:, 1:2], in0=ss[:, 0:1], scalar1=1.0 / 1024.0, scalar2=EPS,
                                                    op0=ALU.mult, op1=ALU.add), reads=[t_ss], writes=[t_ss])
                op("pool", lambda e: e.tensor_tensor(out=ss[:, 2:3], in0=ss[:, 1:2], in1=cm05[:, 0:1], op=ALU.pow), reads=[t_ss, t_c], writes=[t_ss])
                op("dve", lambda e: e.scalar_tensor_tensor(out=tmpf[:], in0=xt[i][:], scalar=ss[:, 2:3], in1=mA[:],
                                                           op0=ALU.mult, op1=ALU.mult), reads=[t_xt[i], t_ss, t_mA], writes=[t_tmpf])
                j = cnt["h"] % 2; cnt["h"] += 1
                op("pool", lambda e: e.tensor_tensor(out=hb[j][:], in0=tmpf[:], in1=mB[:], op=ALU.add),
                   reads=[t_tmpf, t_mB], writes=[t_hb[j]])
                for k in range(8):
                    op("pe", lambda e, k=k: e.transpose(out=psT[:, k, :], in_=hb[j][:, k * 128:(k + 1) * 128], identity=ident_b[:]),
                       reads=[t_hb[j], t_c], writes=[t_psT], inc=(k == 7))
                op("act", lambda e: e.copy(out=dst[:, :, col0:col0 + 128], in_=psT[:]), reads=[t_psT], writes=[t_dst])

            def proj_fm(ps, t_ps, wcol, n, src=None, t_src=None):
                src = hT if src is None else src
                t_src = t_hT if t_src is None else t_src
                for k in range(8):
                    op("pe", lambda e, k=k: e.matmul(ps[:, 0:n], lhsT=Wb[:, k, wcol:wcol + 128], rhs=src[:, k, 0:n],
                                                     start=(k == 0), stop=(k == 7)),
                       reads=[t_W, t_src], writes=[t_ps], inc=(k == 7))

            def proj_tm(ps, t_ps, wcol, ncols, tcol, src=None, t_src=None):
                src = hT if src is None else src
                t_src = t_hT if t_src is None else t_src
                for k in range(8):
                    op("pe", lambda e, k=k: e.matmul(ps[:, 0:ncols], lhsT=src[:, k, tcol:tcol + 128], rhs=Wb[:, k, wcol:wcol + ncols],
                                                     start=(k == 0), stop=(k == 7)),
                       reads=[t_W, t_src], writes=[t_ps], inc=(k == 7))

            with ExitStack() as p2:
                nqT = alloc(p2, "nqT", [128, 2, 2304], BF16); t_nq = Tk("nq")
                nkT = alloc(p2, "nkT", [128, 2, 2816], BF16); t_nk = Tk("nk")
                nVa = alloc(p2, "nVa", [128, 37, 4, 65], BF16); t_nv = Tk("nv")
                poolin = alloc(p2, "poolin", [128, 20, 256], BF16); t_pin = Tk("pin")
                psT = [alloc(p2, "psT%d" % i, [128, 8, 128], BF16, psum=True) for i in range(2)]
                t_psT = [Tk("psT%d" % i) for i in range(2)]
                pb = [alloc(p2, "pb%d" % i, [128, 512], F32, psum=True) for i in range(6)]
                t_pb = [Tk("pb%d" % i) for i in range(6)]
                p2a = ExitStack()
                rq = alloc(p2a, "rq", [128, 2, 512], F32); t_rq = Tk("rq")
                t1 = alloc(p2a, "t1", [128, 512], F32); t_t1 = Tk("t1")
                t2 = alloc(p2a, "t2", [128, 512], F32); t_t2 = Tk("t2")
                zf = alloc(p2a, "zf", [128, 512], F32); t_zf = Tk("zf")
                vn = alloc(p2a, "vn", [128, 256], BF16); t_vn = Tk("vn")
                so = alloc(p2a, "so", [128, 256], F32); t_so = Tk("so")
                sob = alloc(p2a, "sob", [128, 256], BF16); t_sob = Tk("sob")
                gsg = alloc(p2a, "gsg", [128, 256], F32); t_gsg = Tk("gsg")
                bfl = alloc(p2a, "bfl", [128, 2, 256], F32); t_bfl = Tk("bfl")
                wsb = alloc(p2a, "wsb", [128, 2, 4, 128], BF16); t_wsb = Tk("wsb")
                pc = {"pb": 0, "pt": 0}

                def nxt_pb():
                    i = pc["pb"] % 6; pc["pb"] += 1
                    return pb[i], t_pb[i]

                def nxt_pt():
                    i = pc["pt"] % 2; pc["pt"] += 1
                    return psT[i], t_psT[i]

                for (lo, hi, dst) in ((0, 512, 0), (1280, 2048, 512), (2048, 2816, 1280)):
                    dma("pool", Wb[:, :, dst:dst + hi - lo], Wv[:, :, lo:hi], writes=[t_W], multi=(dst > 0))
                dma("sp", gsg[:], g_sgu.to_broadcast([128, 256]), writes=[t_gsg])
                dma("sp", bfl[:], bfull, writes=[t_bfl])
                dma("pool", wsb[:], wsT, writes=[t_wsb])
                op("dve", lambda e: e.memset(nVa[:, :, :, 64:65], 1.0), writes=[t_nv])
                load_mod(mA, t_mA, 0, 0); load_mod(mB, t_mB, 0, 1)

                def sgu_tile(ps, t_ps, v, mcol):
                    op("act", lambda e: e.activation(out=zf[:], in_=ps[:, 0:512], func=AF.Gelu_apprx_tanh), reads=[t_ps], writes=[t_zf])
                    op("act", lambda e: e.activation(out=so[:], in_=zf[:, 256:512], func=AF.Square, accum_out=ss[:, 3:4]),
                       reads=[t_zf], writes=[t_so, t_ss])
                    op("dve", lambda e: e.tensor_scalar(out=ss[:, 4:5], in0=ss[:, 3:4], scalar1=1.0 / 256.0, scalar2=EPS,
                                                        op0=ALU.mult, op1=ALU.add), reads=[t_ss], writes=[t_ss])
                    op("pool", lambda e: e.tensor_tensor(out=ss[:, 5:6], in0=ss[:, 4:5], in1=cm05[:, 0:1], op=ALU.pow), reads=[t_ss, t_c], writes=[t_ss])
                    op("dve", lambda e: e.scalar_tensor_tensor(out=vn[:], in0=zf[:, 256:512], scalar=ss[:, 5:6], in1=gsg[:],
                                                               op0=ALU.mult, op1=ALU.mult), reads=[t_zf, t_ss, t_gsg], writes=[t_vn])
                    sp_, t_sp = nxt_pb()
                    for g in range(4):
                        op("pe", lambda e: e.matmul(sp_[:, g * 64:(g + 1) * 64], lhsT=wsb[:, v, g, :], rhs=vn[:, g * 64:(g + 1) * 64],
                                                    start=True, stop=True), reads=[t_wsb, t_vn], writes=[t_sp], inc=(g == 3))
                    op("dve", lambda e: e.tensor_tensor(out=so[:], in0=sp_[:, 0:256], in1=bfl[:, v, :], op=ALU.add),
                       reads=[t_sp, t_bfl], writes=[t_so])
                    op("pool", lambda e: e.tensor_tensor(out=sob[:], in0=so[:], in1=zf[:, 0:256], op=ALU.mult),
                       reads=[t_so, t_zf], writes=[t_sob])
                    pt, t_pt = nxt_pt()
                    for c2 in range(2):
                        op("pe", lambda e: e.transpose(out=pt[:, c2, :], in_=sob[:, c2 * 128:(c2 + 1) * 128], identity=ident_b[:]),
                           reads=[t_sob, t_c], writes=[t_pt], inc=(c2 == 1))
                    op("act", lambda e: e.copy(out=mixT[:, 4:6, mcol:mcol + 128], in_=pt[:, 0:2, :]),
                       reads=[t_pt], writes=[t_mix[4], t_mix[5]])

                def q_rope(n, qcol, rcol, src=None, t_src=None):
                    dma("sp", rq[:, :, 0:n], ropeQ.rearrange("a p n -> p a n")[:, :, rcol:rcol + n], writes=[t_rq])
                    for ct in range(2):
                        ps_s, t_s = nxt_pb(); proj_fm(ps_s, t_s, 0 + ct * 128, n, src, t_src)
                        ps_r, t_r = nxt_pb(); proj_fm(ps_r, t_r, 256 + ct * 128, n, src, t_src)
                        op("dve", lambda e: e.tensor_tensor(out=t1[:, 0:n], in0=ps_s[:, 0:n], in1=rq[:, 0, 0:n], op=ALU.mult),
                           reads=[t_s, t_rq], writes=[t_t1])
                        op("dve", lambda e: e.tensor_tensor(out=t2[:, 0:n], in0=ps_r[:, 0:n], in1=rq[:, 1, 0:n], op=ALU.mult),
                           reads=[t_r, t_rq], writes=[t_t2])
                        op("pool", lambda e: e.tensor_tensor(out=t1[:, 0:n], in0=t1[:, 0:n], in1=t2[:, 0:n], op=ALU.add),
                           reads=[t_t1, t_t2], writes=[t_t1])
                        sc_ = 32.0 ** -0.5
                        op("dve", lambda e: e.tensor_scalar(out=qA[:, ct, qcol:qcol + n], in0=t1[:, 0:n], scalar1=maskA[:, 0:1],
                                                            scalar2=sc_, op0=ALU.mult, op1=ALU.mult), reads=[t_t1, t_mk], writes=[t_q])
                        op("pool", lambda e: e.tensor_scalar(out=qB[:, ct, qcol:qcol + n], in0=t1[:, 0:n], scalar1=maskA[:, 1:2],
                                                             scalar2=sc_, op0=ALU.mult, op1=ALU.mult), reads=[t_t1, t_mk], writes=[t_q])

                def nq_proj(n, qcol, src=None, t_src=None):
                    for ct in range(2):
                        ps, t_ps = nxt_pb(); proj_fm(ps, t_ps, 1280 + ct * 128, n, src, t_src)
                        op("act", lambda e: e.mul(out=nqT[:, ct, qcol:qcol + n], in_=ps[:, 0:n], mul=0.125), reads=[t_ps], writes=[t_nq])

                def nk_proj(n, kcol, src=None, t_src=None):
                    for ct in range(2):
                        ps, t_ps = nxt_pb(); proj_fm(ps, t_ps, 1536 + ct * 128, n, src, t_src)
                        op("act", lambda e: e.copy(out=nkT[:, ct, kcol:kcol + n], in_=ps[:, 0:n]), reads=[t_ps], writes=[t_nk])

                def nv_proj(tcol, chunk, src=None, t_src=None):
                    ps, t_ps = nxt_pb(); proj_tm(ps, t_ps, 1792, 256, tcol, src, t_src)
                    op("act", lambda e: e.copy(out=nVa[:, chunk, :, 0:64], in_=ps[:, 0:256].rearrange("p (h d) -> p h d", h=4)),
                       reads=[t_ps], writes=[t_nv])

                def pool_proj(tcol, chunk, src=None, t_src=None):
                    ps, t_ps = nxt_pb(); proj_tm(ps, t_ps, 512, 256, tcol, src, t_src)
                    op("act", lambda e: e.copy(out=poolin[:, chunk, :], in_=ps[:, 0:256]), reads=[t_ps], writes=[t_pin])

                def sgu_proj(tcol, v, mcol, src=None, t_src=None):
                    ps, t_ps = nxt_pb(); proj_tm(ps, t_ps, 768, 512, tcol, src, t_src)
                    sgu_tile(ps, t_ps, v, mcol)

                for blk in range(4):
                    for j in range(4):
                        pt, t_pt = nxt_pt()
                        norm_to_hT(x_own[(blk * 4 + j) * 128:(blk * 4 + j + 1) * 128, :], j * 128, pt, t_pt)
                    q_rope(512, blk * 512, blk * 512)
                    nq_proj(512, blk * 512)
                    nk_proj(512, blk * 512)
                    for j in range(4):
                        ch = blk * 4 + j
                        nv_proj(j * 128, ch)
                        if ch <= 14:
                            op("act", lambda e: e.copy(out=nVa[0:64, 16 + ch, :, 0:64], in_=nVa[64:128, ch, :, 0:64]),
                               reads=[t_nv], writes=[t_nv])
                        if ch >= 1:
                            op("act", lambda e: e.copy(out=nVa[64:128, 16 + ch - 1, :, 0:64], in_=nVa[0:64, ch, :, 0:64]),
                               reads=[t_nv], writes=[t_nv])
                        pool_proj(j * 128, ch)
                        sgu_proj(j * 128, 0, ch * 128)
                for j, gt in enumerate((14, 15, 30, 31)):
                    pt, t_pt = nxt_pt()
                    norm_to_hT(Gx[gt * 128:(gt + 1) * 128, :], j * 128, pt, t_pt)
                nk_proj(512, 2048)
                for j in range(4):
                    nv_proj(j * 128, 31 + j)
                pool_proj(128, 16)
                pool_proj(384, 17)
                load_mod(mA, t_mA, 1, 0); load_mod(mB, t_mB, 1, 1)
                for j in range(2):
                    pt, t_pt = nxt_pt()
                    norm_to_hT(xc[j * 128:(j + 1) * 128, :], j * 128, pt, t_pt, dst=hcT, t_dst=t_hcT)
                nk_proj(256, 2560, hcT, t_hcT)
                for j in range(2):
                    nv_proj(j * 128, 35 + j, hcT, t_hcT)
                if upd:
                    q_rope(256, 2048, 2048, hcT, t_hcT)
                    nq_proj(256, 2048, hcT, t_hcT)
                    for j in range(2):
                        pool_proj(j * 128, 18 + j, hcT, t_hcT)
                        sgu_proj(j * 128, 1, 2048 + j * 128, hcT, t_hcT)

                fw.barrier()
                p2a.close()
                chk("proj")
                p2b = ExitStack()
                pooled = alloc(p2b, "pooled", [64, 512], BF16); t_pooled = Tk("pooled")
                pbd = alloc(p2b, "pbd", [128, 52, 128], BF16); t_pbd = Tk("pbd")
                wpl = alloc(p2b, "wpl", [64, 4, 128], BF16); t_wpl = Tk("wpl")
                spl = alloc(p2b, "spl", [128, 2], F32); t_spl = Tk("spl")
                dma("pool", pbd[:], pband, writes=[t_pbd])
                dma("pool", wpl[:], w_pool, writes=[t_wpl])
                dma("sp", spl[:], s_pool, writes=[t_spl])

                def pool_chunk(mats, mcol):
                    p1, t_p1 = nxt_pb()
                    for g in range(4):
                        for mi, (mid, chn) in enumerate(mats(g)):
                            op("pe", lambda e: e.matmul(p1[0:64, g * 128:(g + 1) * 128], lhsT=poolin[:, chn, g * 64:(g + 1) * 64],
                                                        rhs=pbd[:, mid, :], start=(mi == 0), stop=(mi == len(mats(g)) - 1)),
                               reads=[t_pin, t_pbd], writes=[t_p1], inc=(g == 3 and mi == len(mats(g)) - 1))
                    op("act", lambda e: e.copy(out=pooled[:], in_=p1[0:64, :]), reads=[t_p1], writes=[t_pooled])
                    p2_, t_p2 = nxt_pb()
                    for g in range(4):
                        pr = g // 2
                        op("pe", lambda e: e.matmul(p2_[:, pr * 128:(pr + 1) * 128], lhsT=wpl[:, g, :], rhs=pooled[:, g * 128:(g + 1) * 128],
                                                    start=(g % 2 == 0), stop=(g % 2 == 1)), reads=[t_wpl, t_pooled], writes=[t_p2], inc=(g == 3))
                    for pr in range(2):
                        op("dve", lambda e: e.tensor_scalar(out=mixT[:, 2 + pr, mcol:mcol + 128],
                                                            in0=p2_[:, pr * 128:(pr + 1) * 128], scalar1=spl[:, pr:pr + 1], scalar2=None,
                                                            op0=ALU.mult), reads=[t_p2, t_spl], writes=[t_mix[2 + pr]])

                for c in range(16):
                    if c == 0:
                        mats = lambda g: [(9 * g + 0, 0), (9 * g + 1, 1)]
                    elif c == 15:
                        mats = lambda g: [(9 * g + 5, 14), (9 * g + 6, 15), (9 * g + 7, 16), (9 * g + 8, 17)]
                    else:
                        mats = lambda g, c=c: [(9 * g + 2, c - 1), (9 * g + 3, c), (9 * g + 4, c + 1)]
                    pool_chunk(mats, c * 128)
                if upd:
                    pool_chunk(lambda g: [(36 + 4 * g + 0, 18), (36 + 4 * g + 1, 19)], 2048)
                    pool_chunk(lambda g: [(36 + 4 * g + 2, 18), (36 + 4 * g + 3, 19)], 2048 + 128)
                fw.barrier()

                p2b.close()
                chk("pool")
                esn = [alloc(p2, "esn%d" % i, [128, 704], BF16) for i in range(2)]
                t_esn = [Tk("esn%d" % i) for i in range(2)]
                rrow = alloc(p2, "rrow", [128, 1024], F32); t_rrow = Tk("rrow")
                bcs = alloc(p2, "bcs", [64, 1024], F32); t_bcs = Tk("bcs")
                otmp = alloc(p2, "otmp", [64, 512], BF16); t_otmp = Tk("otmp")
                rhi = alloc(p2, "rhi", [128, 512], BF16); rlo = alloc(p2, "rlo", [128, 512], BF16); t_rhl = Tk("rhl")
                tabI = alloc(p2, "tabI", [128, 4, 5, 128], BF16); t_tabI = Tk("tabI")
                tabS = [alloc(p2, "tabS%d" % i, [128, 9, 128], BF16) for i in range(2)]
                t_tabS = [Tk("tabS%d" % i) for i in range(2)]
                ntv = ntab.rearrange("v q (h s k) -> v q h s k", h=4, s=9)
                dma("pool", tabI[0:64], ntv[4][:, :, 0:5, :], writes=[t_tabI])
                dma("pool", tabI[64:128], ntv[4][:, :, 0:5, :], writes=[t_tabI], multi=True)
                spec = {"n": 0}
                tab_for = {}
                for rl in range(32):
                    v = rl if rl < 4 else (4 if rl <= 26 else rl - 22)
                    if v == 4:
                        tab_for[rl] = (tabI, t_tabI)

                def get_tab(rl, h):
                    if rl in tab_for:
                        return tabI[:, h, :, :], t_tabI
                    v = rl if rl < 4 else rl - 22
                    i = spec["n"] % 2; spec["n"] += 1
                    hb2 = 64 * (h % 2)
                    dma("pool", tabS[i][hb2:hb2 + 64], ntv[v][:, h, :, :], writes=[t_tabS[i]])
                    return tabS[i][:, :, :], t_tabS[i]

                def attn_epilogue_simple(acc, t_acc, E1, t_E1, n, out_ap, t_out):
                    op("dve", lambda e: e.reciprocal(out=rrow[64:65, 0:n], in_=acc[64:65, 0:n]), reads=[t_acc], writes=[t_rrow])
                    op("dve", lambda e: e.tensor_copy(out=rhi[64:65, 0:n], in_=rrow[64:65, 0:n]), reads=[t_rrow], writes=[t_rhl])
                    op("dve", lambda e: e.tensor_tensor(out=rlo[64:65, 0:n], in0=rrow[64:65, 0:n], in1=rhi[64:65, 0:n], op=ALU.subtract),
                       reads=[t_rrow, t_rhl], writes=[t_rhl])
                    op("pe", lambda e: e.matmul(E1[0:64, 0:n], lhsT=ones_b[64:65, 0:64], rhs=rhi[64:65, 0:n], start=True, stop=False),
                       reads=[t_rhl, t_c], writes=[t_E1], inc=False)
                    op("pe", lambda e: e.matmul(E1[0:64, 0:n], lhsT=ones_b[64:65, 0:64], rhs=rlo[64:65, 0:n], start=False, stop=True),
                       reads=[t_rhl, t_c], writes=[t_E1])
                    op("act", lambda e: e.copy(out=bcs[:, 0:n], in_=E1[0:64, 0:n]), reads=[t_E1], writes=[t_bcs])
                    op("dve", lambda e: e.tensor_tensor(out=otmp[:, 0:n], in0=acc[0:64, 0:n], in1=bcs[:, 0:n], op=ALU.mult),
                       reads=[t_acc, t_bcs], writes=[t_otmp])
                    op("act", lambda e: e.copy(out=out_ap, in_=otmp[:, 0:n]), reads=[t_otmp], writes=[t_out])

                rowc = {"n": 0}
                for rl_order in [list(range(rg * 8, rg * 8 + 8)) for rg in range(4)]:
                    rg = rl_order[0] // 8
                    for h in range(4):
                        j = h // 2; hb_ = 64 * (h % 2)
                        acc, t_acc = pb[4], t_pb[4]
                        def row_desc(rl):
                            a = min(max(rl - 4, 0), 22)
                            chunks = []
                            for m in range(5):
                                r0 = a + 2 * m
                                vch = (r0 // 2) if r0 % 2 == 0 else 16 + (r0 - 1) // 2
                                chunks.append((r0 * 64, vch, m))
                            if rl >= 28:
                                for m in range(4):
                                    chunks.append((2048 + m * 128, 31 + m, 5 + m))
                            for m in range(2):
                                chunks.append((2560 + m * 128, 35 + m, None))
                            i = rowc["n"] % 2; rowc["n"] += 1
                            return dict(rl=rl, chunks=chunks, i=i)

                        def emit_scores(rd):
                            rl, chunks, i = rd["rl"], rd["chunks"], rd["i"]
                            tab, t_tab = get_tab(rl, h)
                            nch = len(chunks)
                            Sa, t_Sa = pb[2 * i], t_pb[2 * i]
                            Sb, t_Sb = pb[2 * i + 1], t_pb[2 * i + 1]
                            for ci, (kcol, vch, slot) in enumerate(chunks):
                                S_, t_S = (Sa, t_Sa) if ci < 8 else (Sb, t_Sb)
                                cc = (ci % 8) * 64
                                last_in_bank = (ci == min(7, nch - 1)) or (ci == nch - 1)
                                op("pe", lambda e: e.matmul(S_[:, cc:cc + 64], lhsT=nkT[hb_:hb_ + 64, j, kcol:kcol + 128],
                                                            rhs=nqT[hb_:hb_ + 64, j, rl * 64:(rl + 1) * 64], start=True, stop=(slot is None)),
                                   reads=[t_nk, t_nq], writes=[t_S], inc=(slot is None and last_in_bank))
                                if slot is not None:
                                    op("pe", lambda e: e.matmul(S_[:, cc:cc + 64], lhsT=tab[hb_:hb_ + 64, slot, :], rhs=ident_b[hb_:hb_ + 64, hb_:hb_ + 64],
                                                                start=False, stop=True),
                                       reads=[t_tab, t_c], writes=[t_S], inc=last_in_bank)

                        def emit_rest(rr_, rd):
                            rl, chunks, i = rd["rl"], rd["chunks"], rd["i"]
                            nch = len(chunks)
                            Sa, t_Sa = pb[2 * i], t_pb[2 * i]
                            Sb, t_Sb = pb[2 * i + 1], t_pb[2 * i + 1]
                            na = min(nch, 8) * 64
                            op("act", lambda e: e.activation(out=esn[i][:, 0:na], in_=Sa[:, 0:na], func=AF.Exp), reads=[t_Sa], writes=[t_esn[i]])
                            if nch > 8:
                                nb_ = (nch - 8) * 64
                                op("act", lambda e: e.activation(out=esn[i][:, 512:512 + nb_], in_=Sb[:, 0:nb_], func=AF.Exp),
                                   reads=[t_Sb], writes=[t_esn[i]])
                            for ci, (kcol, vch, slot) in enumerate(chunks):
                                op("pe", lambda e: e.matmul(acc[0:65, rr_ * 64:(rr_ + 1) * 64], lhsT=nVa[:, vch, h, :],
                                                            rhs=esn[i][:, ci * 64:(ci + 1) * 64], start=(ci == 0), stop=(ci == nch - 1)),
                                   reads=[t_nv, t_esn[i]], writes=[t_acc], inc=(ci == nch - 1))

                        rds = [row_desc(rl) for rl in rl_order]
                        emit_scores(rds[0])
                        for rr_, rd in enumerate(rds):
                            if rr_ + 1 < len(rds):
                                emit_scores(rds[rr_ + 1])
                            emit_rest(rr_, rd)
                        attn_epilogue_simple(acc, t_acc, pb[5], t_pb[5], 512,
                                             mixT[hb_:hb_ + 64, 6 + j, rg * 512:(rg + 1) * 512], t_mix[6 + j])
                        chk("nbrC")
                if upd:
                    for h in range(4):
                        j = h // 2; hb_ = 64 * (h % 2)
                        Sa, t_Sa = pb[0], t_pb[0]
                        acc, t_acc = pb[4], t_pb[4]
                        for ci in range(2):
                            op("pe", lambda e: e.matmul(Sa[:, ci * 256:(ci + 1) * 256], lhsT=nkT[hb_:hb_ + 64, j, 2560 + ci * 128:2560 + (ci + 1) * 128],
                                                        rhs=nqT[hb_:hb_ + 64, j, 2048:2304], start=True, stop=True),
                               reads=[t_nk, t_nq], writes=[t_Sa], inc=(ci == 1))
                        op("act", lambda e: e.activation(out=esn[0][:, 0:512], in_=Sa[:, 0:512], func=AF.Exp), reads=[t_Sa], writes=[t_esn[0]])
                        for ci in range(2):
                            op("pe", lambda e: e.matmul(acc[0:65, 0:256], lhsT=nVa[:, 35 + ci, h, :], rhs=esn[0][:, ci * 256:(ci + 1) * 256],
                                                        start=(ci == 0), stop=(ci == 1)), reads=[t_nv, t_esn[0]], writes=[t_acc], inc=(ci == 1))
                        attn_epilogue_simple(acc, t_acc, pb[5], t_pb[5], 256, mixT[hb_:hb_ + 64, 6 + j, 2048:2304], t_mix[6 + j])
                fw.barrier()
                chk("nbr")

            with ExitStack() as p1:
                kT = alloc(p1, "kT", [128, 2, 4352], BF16); t_kT = Tk("kT")
                Va = alloc(p1, "Va", [128, 34, 4, 65], BF16); t_Va = Tk("Va")
                p1a = ExitStack()
                psT = [alloc(p1a, "qsT%d" % i, [128, 8, 128], BF16, psum=True) for i in range(2)]
                t_psT = [Tk("qsT%d" % i) for i in range(2)]
                pb = [alloc(p1a, "qb%d" % i, [128, 512], F32, psum=True) for i in range(6)]
                t_pb = [Tk("qb%d" % i) for i in range(6)]
                rk = alloc(p1, "rk", [128, 2, 512], F32); t_rk = Tk("rk")
                t1 = alloc(p1, "u1", [128, 512], F32); t_t1 = Tk("u1")
                t2 = alloc(p1, "u2", [128, 512], F32); t_t2 = Tk("u2")
                pc = {"pb": 0, "pt": 0}

                def nxt_pb():
                    i = pc["pb"] % 6; pc["pb"] += 1
                    return pb[i], t_pb[i]

                def nxt_pt():
                    i = pc["pt"] % 2; pc["pt"] += 1
                    return psT[i], t_psT[i]

                dma("pool", Wb[:, :, 0:768], Wv[:, :, 512:1280], writes=[t_W])
                op("dve", lambda e: e.memset(Va[:, :, :, 64:65], 1.0), writes=[t_Va])
                load_mod(mA, t_mA, 0, 0); load_mod(mB, t_mB, 0, 1)
                rkv = ropeK.rearrange("a p n -> p a n")

                def k_block(n, kcol, src=None, t_src=None):
                    dma("sp", rk[:, :, 0:n], rkv[:, :, kcol:kcol + n], writes=[t_rk])
                    for ct in range(2):
                        ps_s, t_s = nxt_pb(); proj_fm(ps_s, t_s, 0 + ct * 128, n, src, t_src)
                        ps_r, t_r = nxt_pb(); proj_fm(ps_r, t_r, 256 + ct * 128, n, src, t_src)
                        op("dve", lambda e: e.tensor_tensor(out=t1[:, 0:n], in0=ps_s[:, 0:n], in1=rk[:, 0, 0:n], op=ALU.mult),
                           reads=[t_s, t_rk], writes=[t_t1])
                        op("dve", lambda e: e.tensor_tensor(out=t2[:, 0:n], in0=ps_r[:, 0:n], in1=rk[:, 1, 0:n], op=ALU.mult),
                           reads=[t_r, t_rk], writes=[t_t2])
                        op("pool", lambda e: e.tensor_tensor(out=kT[:, ct, kcol:kcol + n], in0=t1[:, 0:n], in1=t2[:, 0:n], op=ALU.add),
                           reads=[t_t1, t_t2], writes=[t_kT])

                def v_tile(tcol, chunk, src=None, t_src=None):
                    ps, t_ps = nxt_pb(); proj_tm(ps, t_ps, 512, 256, tcol, src, t_src)
                    op("act", lambda e: e.copy(out=Va[:, chunk, :, 0:64], in_=ps[:, 0:256].rearrange("p (h d) -> p h d", h=4)),
                       reads=[t_ps], writes=[t_Va])

                for blk in range(8):
                    for j in range(4):
                        pt, t_pt = nxt_pt()
                        norm_to_hT(Gx[(blk * 4 + j) * 128:(blk * 4 + j + 1) * 128, :], j * 128, pt, t_pt)
                    k_block(512, blk * 512)
                    for j in range(4):
                        v_tile(j * 128, blk * 4 + j)
                k_block(256, 4096, hcT, t_hcT)
                for j in range(2):
                    v_tile(j * 128, 32 + j, hcT, t_hcT)
                fw.barrier()
                p1a.close()
                chk("kv")

                pq = [alloc(p1, "pq%d" % i, [128, 512], F32, psum=True) for i in range(8)]
                t_pq = [Tk("pq%d" % i) for i in range(8)]
                es = [alloc(p1, "es%d" % i, [128, 512], BF16) for i in range(4)]
                t_es = [Tk("es%d" % i) for i in range(4)]
                rrow = alloc(p1, "rrow2", [128, 1024], F32); t_rrow = Tk("rrow2")
                bcs = alloc(p1, "bcs2", [64, 1024], F32); t_bcs = Tk("bcs2")
                o1 = alloc(p1, "o1", [64, 512], F32); t_o1 = Tk("o1")
                o2 = alloc(p1, "o2", [64, 512], F32); t_o2 = Tk("o2")
                sq = alloc(p1, "sq", [64, 512], F32); t_sq = Tk("sq")
                rs = alloc(p1, "rs", [64, 512], F32); t_rs = Tk("rs")
                ob = alloc(p1, "ob", [64, 512], BF16); t_ob = Tk("ob")
                rhi = alloc(p1, "rhi2", [128, 1024], BF16); rlo = alloc(p1, "rlo2", [128, 1024], BF16); t_rhl = Tk("rhl2")
                op("dve", lambda e: e.memset(rrow[64:65, :], 1.0), writes=[t_rrow])
                itc = {"n": 0}

                def diff_block(h, qcol, n, kchunks):
                    j = h // 2; hb_ = 64 * (h % 2)
                    acc1, t_a1 = pq[4], t_pq[4]
                    acc2, t_a2 = pq[5], t_pq[5]
                    E1, t_E1 = pq[6], t_pq[6]
                    E2, t_E2 = pq[7], t_pq[7]
                    nk_ = len(kchunks)
                    base_n = itc["n"]; itc["n"] += nk_

                    def bufs(ki):
                        i = (base_n + ki) % 2
                        return (pq[2 * i], t_pq[2 * i], pq[2 * i + 1], t_pq[2 * i + 1], es[2 * i], t_es[2 * i], es[2 * i + 1], t_es[2 * i + 1])

                    def emit_S(ki):
                        kc = kchunks[ki]
                        S1, t_S1, S2, t_S2 = bufs(ki)[0:4]
                        op("pe", lambda e: e.matmul(S1[:, 0:n], lhsT=kT[hb_:hb_ + 64, j, kc * 128:(kc + 1) * 128],
                                                    rhs=qA[hb_:hb_ + 64, j, qcol:qcol + n], start=True, stop=True),
                           reads=[t_kT, t_q], writes=[t_S1])
                        op("pe", lambda e: e.matmul(S2[:, 0:n], lhsT=kT[hb_:hb_ + 64, j, kc * 128:(kc + 1) * 128],
                                                    rhs=qB[hb_:hb_ + 64, j, qcol:qcol + n], start=True, stop=True),
                           reads=[t_kT, t_q], writes=[t_S2])

                    emit_S(0)
                    for ki, kc in enumerate(kchunks):
                        if ki + 1 < nk_:
                            emit_S(ki + 1)
                        S1, t_S1, S2, t_S2, e1, t_e1, e2, t_e2 = bufs(ki)
                        op("act", lambda e: e.activation(out=e1[:, 0:n], in_=S1[:, 0:n], func=AF.Exp), reads=[t_S1], writes=[t_e1])
                        op("act", lambda e: e.activation(out=e2[:, 0:n], in_=S2[:, 0:n], func=AF.Exp), reads=[t_S2], writes=[t_e2])
                        op("pe", lambda e: e.matmul(acc1[0:65, 0:n], lhsT=Va[:, kc, h, :], rhs=e1[:, 0:n], start=(ki == 0), stop=(ki == nk_ - 1)),
                           reads=[t_Va, t_e1], writes=[t_a1])
                        op("pe", lambda e: e.matmul(acc2[0:65, 0:n], lhsT=Va[:, kc, h, :], rhs=e2[:, 0:n], start=(ki == 0), stop=(ki == nk_ - 1)),
                           reads=[t_Va, t_e2], writes=[t_a2])
                    op("dve", lambda e: e.reciprocal(out=rrow[64:65, 0:n], in_=acc1[64:65, 0:n]), reads=[t_a1], writes=[t_rrow])
                    op("dve", lambda e: e.reciprocal(out=rrow[64:65, 512:512 + n], in_=acc2[64:65, 0:n]), reads=[t_a2], writes=[t_rrow])
                    op("dve", lambda e: e.tensor_scalar(out=rrow[64:65, 512:512 + n], in0=rrow[64:65, 512:512 + n], scalar1=nlam[64:65, 3:4],
                                                        scalar2=None, op0=ALU.mult), reads=[t_rrow, t_nlam], writes=[t_rrow])
                    op("dve", lambda e: e.tensor_copy(out=rhi[64:65, :], in_=rrow[64:65, :]), reads=[t_rrow], writes=[t_rhl])
                    op("dve", lambda e: e.tensor_tensor(out=rlo[64:65, :], in0=rrow[64:65, :], in1=rhi[64:65, :], op=ALU.subtract),
                       reads=[t_rrow, t_rhl], writes=[t_rhl])
                    for (E_, t_E, c0) in ((E1, t_E1, 0), (E2, t_E2, 512)):
                        op("pe", lambda e: e.matmul(E_[0:64, 0:n], lhsT=ones_b[64:65, 0:64], rhs=rhi[64:65, c0:c0 + n], start=True, stop=False),
                           reads=[t_rhl, t_c], writes=[t_E], inc=False)
                        op("pe", lambda e: e.matmul(E_[0:64, 0:n], lhsT=ones_b[64:65, 0:64], rhs=rlo[64:65, c0:c0 + n], start=False, stop=True),
                           reads=[t_rhl, t_c], writes=[t_E])
                    op("act", lambda e: e.copy(out=bcs[:, 0:n], in_=E1[0:64, 0:n]), reads=[t_E1], writes=[t_bcs])
                    op("act", lambda e: e.copy(out=bcs[:, 512:512 + n], in_=E2[0:64, 0:n]), reads=[t_E2], writes=[t_bcs])
                    op("dve", lambda e: e.tensor_tensor(out=o1[:, 0:n], in0=acc1[0:64, 0:n], in1=bcs[:, 0:n], op=ALU.mult),
                       reads=[t_a1, t_bcs], writes=[t_o1])
                    op("dve", lambda e: e.tensor_tensor(out=o2[:, 0:n], in0=acc2[0:64, 0:n], in1=bcs[:, 512:512 + n], op=ALU.mult),
                       reads=[t_a2, t_bcs], writes=[t_o2])
                    op("pool", lambda e: e.tensor_tensor(out=o1[:, 0:n], in0=o1[:, 0:n], in1=o2[:, 0:n], op=ALU.add),
                       reads=[t_o1, t_o2], writes=[t_o1])
                    op("act", lambda e: e.activation(out=sq[:, 0:n], in_=o1[:, 0:n], func=AF.Square), reads=[t_o1], writes=[t_sq])
                    op("pe", lambda e: e.matmul(E1[0:64, 0:n], lhsT=od64[0:64, 0:64], rhs=sq[:, 0:n], start=True, stop=True),
                       reads=[t_sq, t_c], writes=[t_E1])
                    op("dve", lambda e: e.tensor_scalar(out=rs[:, 0:n], in0=E1[0:64, 0:n], scalar1=EPS, scalar2=None, op0=ALU.add),
                       reads=[t_E1], writes=[t_rs])
                    op("act", lambda e: e.activation(out=sq[:, 0:n], in_=rs[:, 0:n], func=AF.Sqrt), reads=[t_rs], writes=[t_sq])
                    op("dve", lambda e: e.reciprocal(out=sq[:, 0:n], in_=sq[:, 0:n]), reads=[t_sq], writes=[t_sq])
                    op("dve", lambda e: e.scalar_tensor_tensor(out=ob[:, 0:n], in0=o1[:, 0:n], scalar=gs1[0:64, 0:1],
                                                               in1=sq[:, 0:n], op0=ALU.mult, op1=ALU.mult),
                       reads=[t_o1, t_gs1, t_sq], writes=[t_ob])
                    op("act", lambda e: e.copy(out=mixT[hb_:hb_ + 64, j, qcol:qcol + n], in_=ob[:, 0:n]), reads=[t_ob], writes=[t_mix[j]])

                for h in range(4):
                    for qb in range(4):
                        diff_block(h, qb * 512, 512, list(range(34)))
                    if upd:
                        diff_block(h, 2048, 256, [32, 33])
                fw.barrier()
                chk("diff")

            with ExitStack() as p3:
                wo = alloc(p3, "wo", [128, 8, 1024], BF16); t_wo = Tk("wo")
                po = [alloc(p3, "po%d" % i, [128, 512], F32, psum=True) for i in range(4)]
                t_po = [Tk("po%d" % i) for i in range(4)]
                xm = [alloc(p3, "xm%d" % i, [128, 1024], F32) for i in range(2)]
                t_xm = [Tk("xm%d" % i) for i in range(2)]
                dma("pool", wo[:], w_out.rearrange("(k p) c -> p k c", p=128), writes=[t_wo])
                if dbg:
                    dtile = alloc(p3, "dtile", [128, 2304], F32); t_dt = Tk("dtile")
                    for f in range(8):
                        op("dve", lambda e: e.tensor_copy(out=dtile[:], in_=mixT[:, f, :]), reads=[t_mix[f]], writes=[t_dt])
                        dma("sp", dbg_out["d_mix"][f * 128:(f + 1) * 128, :], dtile[:], reads=[t_dt])
                ntile = 18 if upd else 16
                for i in range(ntile):
                    if i == 0:
                        load_mod(mA, t_mA, 0, 2)
                    if i == 16:
                        load_mod(mA, t_mA, 1, 2)
                    src = x_own[i * 128:(i + 1) * 128, :] if i < 16 else xc[(i - 16) * 128:(i - 15) * 128, :]
                    b = i % 2
                    dma("sp", xt[b][:], src, writes=[t_xt[b]])
                    for half in range(2):
                        p_, t_p = po[(2 * i + half) % 4], t_po[(2 * i + half) % 4]
                        for f in range(8):
                            op("pe", lambda e: e.matmul(p_[:, :], lhsT=mixT[:, f, i * 128:(i + 1) * 128], rhs=wo[:, f, half * 512:(half + 1) * 512],
                                                        start=(f == 0), stop=(f == 7)), reads=[t_mix[f], t_wo], writes=[t_p], inc=(f == 7))
                        hs = slice(half * 512, (half + 1) * 512)
                        op("dve", lambda e: e.tensor_tensor(out=xm[b][:, hs], in0=p_[:, :], in1=mA[:, hs], op=ALU.mult),
                           reads=[t_p, t_mA], writes=[t_xm[b]])
                    op("pool", lambda e: e.tensor_tensor(out=xm[b][:], in0=xm[b][:], in1=xt[b][:], op=ALU.add),
                       reads=[t_xm[b], t_xt[b]], writes=[t_xm[b]])
                    dma("sp", xmid[i * 128:(i + 1) * 128, :], xm[b][:], reads=[t_xm[b]], writes=[t_xmid], multi=(i > 0))
                    if dbg:
                        dma("sp", dbg_out["d_xmid"][i * 128:(i + 1) * 128, :], xm[b][:], reads=[t_xm[b]])
                fw.barrier()
                chk("xmid")

        IOA = bass.IndirectOffsetOnAxis
        with ExitStack() as mo:
            hf_b = alloc(mo, "hf_b", [128, NTT, 1024], BF16); t_hf = [Tk("hf%d" % i) for i in range(NTT)]
            gk = alloc(mo, "gk", [128, NTT, 4], F32); t_gk = Tk("gk")
            dest_i = alloc(mo, "dest_i", [128, NTT, 4], I32); t_di = Tk("dest_i")
            widx = alloc(mo, "widx", [128, NB], I32); t_widx = Tk("widx")
            eidx = alloc(mo, "eidx", [128, NB], I32); t_eidx = Tk("eidx")
            mA = alloc(mo, "mA2", [128, 1024], F32); t_mA = Tk("mA2")
            mB = alloc(mo, "mB2", [128, 1024], F32); t_mB = Tk("mB2")
            xt = [alloc(mo, "yt%d" % i, [128, 1024], F32) for i in range(2)]
            t_xt = [Tk("yt%d" % i) for i in range(2)]
            ss = alloc(mo, "ss2", [128, 8], F32); t_ss = Tk("ss2")
            with ExitStack() as rt:
                masks = alloc(rt, "masks", [128, NTT, 32], F32); t_mk = Tk("masks")
                mask_b = alloc(rt, "mask_b", [128, NTT, 32], BF16)
                gates = alloc(rt, "gates", [128, NTT, 32], F32); t_gt = Tk("gates")
                wr = alloc(rt, "wr", [128, 8, 32], F32); t_wr = Tk("wr")
                br = alloc(rt, "br", [128, 32], F32); t_br = Tk("br")
                hf32 = alloc(rt, "hf32", [128, 1024], F32); t_hf32 = Tk("hf32")
                tmpf = alloc(rt, "tmpf2", [128, 1024], F32); t_tmpf = Tk("tmpf2")
                hfT = alloc(rt, "hfT32", [128, 8, 128], F32); t_hfT = Tk("hfT32")
                pTa = alloc(rt, "pTa", [128, 4, 128], F32, psum=True); t_pTa = Tk("pTa")
                pTb = alloc(rt, "pTb", [128, 4, 128], F32, psum=True); t_pTb = Tk("pTb")
                pl = [alloc(rt, "pl%d" % i, [128, 32], F32, psum=True) for i in range(2)]
                t_pl = [Tk("pl%d" % i) for i in range(2)]
                pcn = alloc(rt, "pcn", [128, 32], F32, psum=True); t_pcn = Tk("pcn")
                sm = alloc(rt, "sm", [128, 16, 32], F32); t_sm = Tk("sm")
                t8 = alloc(rt, "t8", [128, 16], F32); t_t8 = Tk("t8")
                tri = alloc(rt, "tri", [128, 128], BF16); t_tri = Tk("tri")
                be = alloc(rt, "be", [128, NB], F32); t_be = Tk("be")
                wf = alloc(rt, "wf", [128, NB], F32); t_wf = Tk("wf")
                pid = alloc(rt, "pid", [128, 1], F32); t_pid = Tk("pid")
                zt = alloc(rt, "zt", [128, 8, 1024], BF16); t_zt = Tk("zt")
                dma("sp", wr[:], w_router.rearrange("(k p) e -> p k e", p=128), writes=[t_wr])
                dma("sp", br[:], b_router.to_broadcast([128, 32]), writes=[t_br])
                op("pool", lambda e: e.memset(tri[:], 1.0), writes=[t_tri])
                op("pool", lambda e: e.affine_select(out=tri[:], in_=tri[:], pattern=[[1, 128]], compare_op=ALU.is_gt, fill=0.0,
                                                     base=0, channel_multiplier=-1), reads=[t_tri], writes=[t_tri])
                pid_i = alloc(rt, "pid_i", [128, 1], I32)
                op("pool", lambda e: e.iota(pid_i[:], pattern=[[0, 1]], base=0, channel_multiplier=1), writes=[t_pid])
                op("pool", lambda e: e.tensor_copy(out=pid[:], in_=pid_i[:]), reads=[t_pid], writes=[t_pid])
                op("pool", lambda e: e.memset(zt[:], 0.0), writes=[t_zt])
                xsz = xs_d.rearrange("(n p) c -> p n c", p=128)
                for z in range(NB * 2 // 8):
                    dma("sp", xsz[:, z * 8:(z + 1) * 8, :], zt[:], reads=[t_zt], writes=[t_xs], multi=(z > 0))
                for i in range(NTT):
                    if i == 0:
                        load_mod(mA, t_mA, 0, 3); load_mod(mB, t_mB, 0, 4)
                    if i == 16:
                        load_mod(mA, t_mA, 1, 3); load_mod(mB, t_mB, 1, 4)
                    b = i % 2
                    dma("sp", xt[b][:], xmid[i * 128:(i + 1) * 128, :], reads=[t_xmid], writes=[t_xt[b]])
                    op("act", lambda e: e.activation(out=tmpf[:], in_=xt[b][:], func=AF.Square, accum_out=ss[:, 0:1]),
                       reads=[t_xt[b]], writes=[t_tmpf, t_ss])
                    op("dve", lambda e: e.tensor_scalar(out=ss[:, 1:2], in0=ss[:, 0:1], scalar1=1.0 / 1024.0, scalar2=EPS,
                                                        op0=ALU.mult, op1=ALU.add), reads=[t_ss], writes=[t_ss])
                    op("pool", lambda e: e.tensor_tensor(out=ss[:, 2:3], in0=ss[:, 1:2], in1=cm05[:, 0:1], op=ALU.pow), reads=[t_ss, t_c], writes=[t_ss])
                    op("dve", lambda e: e.scalar_tensor_tensor(out=tmpf[:], in0=xt[b][:], scalar=ss[:, 2:3], in1=mA[:],
                                                               op0=ALU.mult, op1=ALU.mult), reads=[t_xt[b], t_ss, t_mA], writes=[t_tmpf])
                    op("pool", lambda e: e.tensor_tensor(out=hf32[:], in0=tmpf[:], in1=mB[:], op=ALU.add),
                       reads=[t_tmpf, t_mB], writes=[t_hf32])
                    op("act", lambda e: e.copy(out=hf_b[:, i, :], in_=hf32[:]), reads=[t_hf32], writes=[t_hf[i]])
                    for k in range(8):
                        pt_, t_pt = (pTa, t_pTa) if k < 4 else (pTb, t_pTb)
                        op("pe", lambda e: e.transpose(out=pt_[:, k % 4, :], in_=hf32[:, k * 128:(k + 1) * 128], identity=ident_f[:]),
                           reads=[t_hf32, t_c], writes=[t_pt], inc=(k % 4 == 3))
                    op("act", lambda e: e.copy(out=hfT[:, 0:4, :], in_=pTa[:]), reads=[t_pTa], writes=[t_hfT])
                    op("dve", lambda e: e.tensor_copy(out=hfT[:, 4:8, :], in_=pTb[:]), reads=[t_pTb], writes=[t_hfT])
                    p_, t_p = pl[b], t_pl[b]
                    for k in range(8):
                        op("pe", lambda e: e.matmul(p_[:, :], lhsT=hfT[:, k, :], rhs=wr[:, k, :], start=(k == 0), stop=(k == 7)),
                           reads=[t_hfT, t_wr], writes=[t_p], inc=(k == 7))
                    lg = sm[:, 0, :]
                    op("dve", lambda e: e.tensor_tensor(out=lg, in0=p_[:, :], in1=br[:], op=ALU.add), reads=[t_p, t_br], writes=[t_sm])
                    if dbg:
                        dma("sp", dbg_out["d_log"][i * 128:(i + 1) * 128, :], lg, reads=[t_sm])
                    op("dve", lambda e: e.max(out=t8[:, 0:8], in_=lg), reads=[t_sm], writes=[t_t8])
                    op("dve", lambda e: e.tensor_scalar(out=masks[:, i, :], in0=lg, scalar1=t8[:, 3:4], scalar2=None, op0=ALU.is_ge),
                       reads=[t_sm, t_t8], writes=[t_mk])
                    op("dve", lambda e: e.tensor_copy(out=mask_b[:, i, :], in_=masks[:, i, :]), reads=[t_mk], writes=[t_mk])
                    op("dve", lambda e: e.tensor_scalar(out=t8[:, 8:9], in0=t8[:, 0:1], scalar1=-1.0, scalar2=None, op0=ALU.mult),
                       reads=[t_t8], writes=[t_t8])
                    op("act", lambda e: e.activation(out=sm[:, 1, :], in_=lg, func=AF.Exp, bias=t8[:, 8:9], scale=1.0),
                       reads=[t_sm, t_t8], writes=[t_sm])
                    op("dve", lambda e: e.tensor_tensor(out=sm[:, 1, :], in0=sm[:, 1, :], in1=masks[:, i, :], op=ALU.mult),
                       reads=[t_sm, t_mk], writes=[t_sm])
                    op("dve", lambda e: e.reduce_sum(out=t8[:, 9:10], in_=sm[:, 1, :], axis=AX.X), reads=[t_sm], writes=[t_t8])
                    op("dve", lambda e: e.reciprocal(out=t8[:, 10:11], in_=t8[:, 9:10]), reads=[t_t8], writes=[t_t8])
                    op("dve", lambda e: e.tensor_scalar(out=gates[:, i, :], in0=sm[:, 1, :], scalar1=t8[:, 10:11], scalar2=None, op0=ALU.mult),
                       reads=[t_sm, t_t8], writes=[t_gt])
                if dbg:
                    dma("sp", dbg_out["d_mask"], masks[:].rearrange("p a b -> p (a b)"), reads=[t_mk])
                    dma("pool", dbg_out["d_maskb"], mask_b[:].rearrange("p a b -> p (a b)"), reads=[t_mk])
                for i in range(NTT):
                    op("pe", lambda e: e.matmul(pcn[:, :], lhsT=ones_b[:, 0:128], rhs=mask_b[:, i, :], start=(i == 0), stop=(i == NTT - 1)),
                       reads=[t_mk, t_c], writes=[t_pcn], inc=(i == NTT - 1))
                c1, m1, pad, ia, ib, ps1 = (sm[:, q, :] for q in (2, 3, 4, 5, 6, 7))
                op("dve", lambda e: e.tensor_copy(out=c1, in_=pcn[:, :]), reads=[t_pcn], writes=[t_sm])
                op("dve", lambda e: e.memset(pad, 0.0), writes=[t_sm])
                for m_ in range((NTT * 128 + BLK - 1) // BLK + 1):
                    op("dve", lambda e: e.tensor_scalar(out=m1, in0=c1, scalar1=float(BLK * m_), scalar2=float(BLK), op0=ALU.is_gt, op1=ALU.mult),
                       reads=[t_sm], writes=[t_sm])
                    op("dve", lambda e: e.tensor_tensor(out=pad, in0=pad, in1=m1, op=ALU.add), reads=[t_sm], writes=[t_sm])
                op("dve", lambda e: e.tensor_copy(out=ia, in_=pad), reads=[t_sm], writes=[t_sm])
                cur, oth = ia, ib
                for s_ in (1, 2, 4, 8, 16):
                    op("dve", lambda e: e.tensor_copy(out=oth[:, 0:s_], in_=cur[:, 0:s_]), reads=[t_sm], writes=[t_sm])
                    op("dve", lambda e: e.tensor_tensor(out=oth[:, s_:32], in0=cur[:, s_:32], in1=cur[:, 0:32 - s_], op=ALU.add),
                       reads=[t_sm], writes=[t_sm])
                    cur, oth = oth, cur
                incl = cur
                op("dve", lambda e: e.tensor_tensor(out=ps1, in0=incl, in1=pad, op=ALU.subtract), reads=[t_sm], writes=[t_sm])
                op("dve", lambda e: e.tensor_scalar(out=ps1, in0=ps1, scalar1=1.0, scalar2=None, op0=ALU.add), reads=[t_sm], writes=[t_sm])
                for b in range(NB):
                    op("dve", lambda e: e.tensor_scalar(out=sm[:, 8, :], in0=incl, scalar1=float(BLK * b), scalar2=None, op0=ALU.is_le),
                       reads=[t_sm], writes=[t_sm])
                    op("dve", lambda e: e.reduce_sum(out=be[:, b:b + 1], in_=sm[:, 8, :], axis=AX.X), reads=[t_sm], writes=[t_be])
                op("dve", lambda e: e.tensor_scalar(out=be[:], in0=be[:], scalar1=31.0, scalar2=None, op0=ALU.min), reads=[t_be], writes=[t_be])
                op("dve", lambda e: e.tensor_copy(out=eidx[:], in_=be[:]), reads=[t_be], writes=[t_eidx])
                op("dve", lambda e: e.tensor_scalar(out=wf[:], in0=be[:], scalar1=128.0, scalar2=pid[:, 0:1], op0=ALU.mult, op1=ALU.add),
                   reads=[t_be, t_pid], writes=[t_wf])
                op("dve", lambda e: e.tensor_copy(out=widx[:], in_=wf[:]), reads=[t_wf], writes=[t_widx])
                fw._need(fw.E["pool"], t_xs.w)
                for i in range(NTT):
                    p_, t_p = pl[i % 2], t_pl[i % 2]
                    op("pe", lambda e: e.matmul(p_[:, :], lhsT=tri[:], rhs=mask_b[:, i, :], start=True, stop=(i == 0)),
                       reads=[t_tri, t_mk], writes=[t_p], inc=(i == 0))
                    for i2 in range(i):
                        op("pe", lambda e: e.matmul(p_[:, :], lhsT=ones_b[:, 0:128], rhs=mask_b[:, i2, :], start=False, stop=(i2 == i - 1)),
                           reads=[t_mk, t_c], writes=[t_p], inc=(i2 == i - 1))
                    dm = sm[:, 9, :]
                    op("dve", lambda e: e.tensor_tensor(out=dm, in0=p_[:, :], in1=ps1, op=ALU.add), reads=[t_p, t_sm], writes=[t_sm])
                    op("dve", lambda e: e.tensor_tensor(out=dm, in0=dm, in1=masks[:, i, :], op=ALU.mult), reads=[t_sm, t_mk], writes=[t_sm])
                    op("dve", lambda e: e.tensor_scalar(out=dm, in0=dm, scalar1=-1.0, scalar2=None, op0=ALU.add), reads=[t_sm], writes=[t_sm])
                    if dbg:
                        dma("sp", dbg_out["d_dm"][i * 128:(i + 1) * 128, :], dm, reads=[t_sm])
                        if i == 0:
                            dma("sp", dbg_out["d_sm"], sm[:].rearrange("p a b -> p (a b)"), reads=[t_sm])
                    op("dve", lambda e: e.max(out=t8[:, 0:8], in_=dm), reads=[t_sm], writes=[t_t8])
                    op("dve", lambda e: e.tensor_copy(out=dest_i[:, i, :], in_=t8[:, 0:4]), reads=[t_t8], writes=[t_di])
                    if dbg:
                        dma("sp", dbg_out["d_dest"][i * 128:(i + 1) * 128, :], t8[:, 0:8], reads=[t_t8])
                    for k in range(4):
                        op("dve", lambda e: e.tensor_scalar(out=sm[:, 10, :], in0=dm, scalar1=t8[:, k:k + 1], scalar2=None, op0=ALU.is_equal),
                           reads=[t_sm, t_t8], writes=[t_sm])
                        op("dve", lambda e: e.tensor_tensor(out=sm[:, 10, :], in0=sm[:, 10, :], in1=gates[:, i, :], op=ALU.mult),
                           reads=[t_sm, t_gt], writes=[t_sm])
                        op("dve", lambda e: e.reduce_sum(out=gk[:, i, k:k + 1], in_=sm[:, 10, :], axis=AX.X), reads=[t_sm], writes=[t_gk])
                    for k in range(4):
                        fw.idma(xs_d, IOA(ap=dest_i[:, i, k:k + 1], axis=0), hf_b[:, i, :], None,
                                reads=[t_di, t_hf[i]], writes=[t_xs], multi=True)
                fw.barrier()
                chk("route")

            with ExitStack() as eb:
                wg = [alloc(eb, "wg%d" % i, [128, 8, 2048], BF16) for i in range(2)]
                t_wg = [Tk("wg%d" % i) for i in range(2)]
                wd = [alloc(eb, "wd%d" % i, [128, 8, 1024], BF16) for i in range(2)]
                t_wd = [Tk("wd%d" % i) for i in range(2)]
                bg = [alloc(eb, "bg%d" % i, [2, 2048], BF16) for i in range(2)]
                t_bg = [Tk("bg%d" % i) for i in range(2)]
                bd = [alloc(eb, "bd%d" % i, [2, 1024], BF16) for i in range(2)]
                t_bd = [Tk("bd%d" % i) for i in range(2)]
                xsb = [alloc(eb, "xsb%d" % i, [128, 2, 1024], BF16) for i in range(2)]
                t_xsb = [Tk("xsb%d" % i) for i in range(2)]
                xT = alloc(eb, "xT", [128, 8, 256], BF16); t_xT = Tk("xT")
                act = alloc(eb, "act", [128, 8, 256], BF16); t_act = Tk("act")
                ybs = [alloc(eb, "ybs%d" % i, [128, 2, 1024], F32) for i in range(2)]
                t_ybs = [Tk("ybs%d" % i) for i in range(2)]
                gtt = [alloc(eb, "gtt%d" % i, [128, 256], F32) for i in range(2)]
                t_gtt = [Tk("gtt%d" % i) for i in range(2)]
                sgt = [alloc(eb, "sgt%d" % i, [128, 256], F32) for i in range(2)]
                t_sgt = [Tk("sgt%d" % i) for i in range(2)]
                upt = [alloc(eb, "upt%d" % i, [128, 256], F32) for i in range(2)]
                t_upt = [Tk("upt%d" % i) for i in range(2)]
                pT = [alloc(eb, "mT%d" % i, [128, 8, 128], BF16, psum=True) for i in range(2)]
                t_pT = [Tk("mT%d" % i) for i in range(2)]
                pg = [alloc(eb, "pg%d" % i, [128, 512], F32, psum=True) for i in range(6)]
                t_pg = [Tk("pg%d" % i) for i in range(6)]
                xsv = xs_d.rearrange("(b s p) c -> b p s c", s=2, p=128)
                ybv = yb_d.rearrange("(b s p) c -> b p s c", s=2, p=128)
                fw._need(fw.E["sp"], t_xs.w)

                def load_block(b):
                    i = b % 2
                    fw.idma(wg[i][:].rearrange("p k f -> p (k f)"), None, w_gu, IOA(ap=widx[:, b:b + 1], axis=0),
                            reads=[t_widx], writes=[t_wg[i]])
                    fw.idma(wd[i][:].rearrange("p k f -> p (k f)"), None, w_down, IOA(ap=widx[:, b:b + 1], axis=0),
                            reads=[t_widx], writes=[t_wd[i]])
                    fw.idma(bg[i][0:2, :], None, b_gu, IOA(ap=eidx[0:2, b:b + 1], axis=0), reads=[t_eidx], writes=[t_bg[i]])
                    fw.idma(bd[i][0:2, :], None, b_down, IOA(ap=eidx[0:2, b:b + 1], axis=0), reads=[t_eidx], writes=[t_bd[i]])
                    dma("sp", xsb[i][:], xsv[b], writes=[t_xsb[i]])

                load_block(0)
                for b in range(NB):
                    i = b % 2
                    if b + 1 < NB:
                        load_block(b + 1)
                    for sc_ in range(2):
                        pt_, t_pt = pT[sc_], t_pT[sc_]
                        for k in range(8):
                            op("pe", lambda e: e.transpose(out=pt_[:, k, :], in_=xsb[i][:, sc_, k * 128:(k + 1) * 128], identity=ident_b[:]),
                               reads=[t_xsb[i], t_c], writes=[t_pt], inc=(k == 7))
                        op("act" if sc_ == 0 else "dve",
                           (lambda e: e.copy(out=xT[:, :, 0:128], in_=pt_[:])) if sc_ == 0 else
                           (lambda e: e.tensor_copy(out=xT[:, :, 128:256], in_=pt_[:])), reads=[t_pt], writes=[t_xT])
                    for jj in range(8):
                        q = jj % 2
                        g_, t_g = pg[2 * q], t_pg[2 * q]
                        u_, t_u = pg[2 * q + 1], t_pg[2 * q + 1]
                        for (dst, t_dst, c0) in ((g_, t_g, jj * 128), (u_, t_u, 1024 + jj * 128)):
                            for k in range(8):
                                op("pe", lambda e: e.matmul(dst[:, 0:256], lhsT=wg[i][:, k, c0:c0 + 128], rhs=xT[:, k, :], start=(k == 0), stop=False),
                                   reads=[t_wg[i], t_xT], writes=[t_dst], inc=False)
                            op("pe", lambda e: e.matmul(dst[:, 0:256], lhsT=bg[i][0:1, c0:c0 + 128], rhs=ones_b[0:1, 0:256], start=False, stop=True),
                               reads=[t_bg[i], t_c], writes=[t_dst])
                        op("dve", lambda e: e.tensor_scalar(out=gtt[q][:], in0=g_[:, 0:256], scalar1=7.0, scalar2=None, op0=ALU.min),
                           reads=[t_g], writes=[t_gtt[q]])
                        op("act", lambda e: e.activation(out=sgt[q][:], in_=gtt[q][:], func=AF.Sigmoid, scale=1.702),
                           reads=[t_gtt[q]], writes=[t_sgt[q]])
                        op("dve", lambda e: e.tensor_scalar(out=upt[q][:], in0=u_[:, 0:256], scalar1=7.0, scalar2=-7.0, op0=ALU.min, op1=ALU.max),
                           reads=[t_u], writes=[t_upt[q]])
                        op("dve", lambda e: e.tensor_tensor(out=gtt[q][:], in0=gtt[q][:], in1=sgt[q][:], op=ALU.mult),
                           reads=[t_gtt[q], t_sgt[q]], writes=[t_gtt[q]])
                        op("dve", lambda e: e.scalar_tensor_tensor(out=act[:, jj, :], in0=upt[q][:], scalar=1.0, in1=gtt[q][:],
                                                                    op0=ALU.add, op1=ALU.mult), reads=[t_upt[q], t_gtt[q]], writes=[t_act])
                    for sc_ in range(2):
                        for half in range(2):
                            p_, t_p = pg[4 + half], t_pg[4 + half]
                            for jj in range(8):
                                op("pe", lambda e: e.matmul(p_[:, :], lhsT=act[:, jj, sc_ * 128:(sc_ + 1) * 128], rhs=wd[i][:, jj, half * 512:(half + 1) * 512],
                                                            start=(jj == 0), stop=False), reads=[t_act, t_wd[i]], writes=[t_p], inc=False)
                            op("pe", lambda e: e.matmul(p_[:, :], lhsT=ones_b[0:1, 0:128], rhs=bd[i][0:1, half * 512:(half + 1) * 512], start=False, stop=True),
                               reads=[t_bd[i], t_c], writes=[t_p])
                            if half == 0:
                                op("act", lambda e: e.copy(out=ybs[i][:, sc_, 0:512], in_=p_[:, :]), reads=[t_p], writes=[t_ybs[i]])
                            else:
                                op("dve", lambda e: e.tensor_copy(out=ybs[i][:, sc_, 512:1024], in_=p_[:, :]), reads=[t_p], writes=[t_ybs[i]])
                    dma("sp", ybv[b], ybs[i][:], reads=[t_ybs[i]], writes=[t_yb], multi=(b > 0))
                fw.barrier()
                chk("blocks")

            with ExitStack() as cb:
                yg = [alloc(cb, "yg%d" % i, [128, 4, 1024], F32) for i in range(2)]
                t_yg = [Tk("yg%d" % i) for i in range(2)]
                acc = alloc(cb, "cacc", [128, 1024], F32); t_acc = Tk("cacc")
                xo = [alloc(cb, "xo%d" % i, [128, 1024], F32) for i in range(2)]
                t_xo = [Tk("xo%d" % i) for i in range(2)]
                gf = alloc(cb, "gf", [128, 1024], F32); t_gf = Tk("gf")
                if last:
                    dma("sp", gf[:], g_fin.to_broadcast([128, 1024]), writes=[t_gf])
                for i in range(NTT):
                    if i == 0:
                        load_mod(mA, t_mA, 0, 5)
                    if i == 16:
                        load_mod(mA, t_mA, 1, 5)
                    b = i % 2
                    dma("sp", xt[b][:], xmid[i * 128:(i + 1) * 128, :], reads=[t_xmid], writes=[t_xt[b]])
                    for k in range(4):
                        fw.idma(yg[b][:, k, :], None, yb_d, IOA(ap=dest_i[:, i, k:k + 1], axis=0),
                                reads=[t_di, t_yb], writes=[t_yg[b]], multi=(k > 0))
                    op("dve", lambda e: e.tensor_scalar(out=acc[:], in0=yg[b][:, 0, :], scalar1=gk[:, i, 0:1], scalar2=None, op0=ALU.mult),
                       reads=[t_yg[b], t_gk], writes=[t_acc])
                    for k in range(1, 4):
                        op("dve",
                           lambda e: e.scalar_tensor_tensor(out=acc[:], in0=yg[b][:, k, :], scalar=gk[:, i, k:k + 1], in1=acc[:],
                                                            op0=ALU.mult, op1=ALU.add), reads=[t_yg[b], t_gk, t_acc], writes=[t_acc])
                    op("dve", lambda e: e.tensor_tensor(out=acc[:], in0=acc[:], in1=mA[:], op=ALU.mult), reads=[t_acc, t_mA], writes=[t_acc])
                    op("pool", lambda e: e.tensor_tensor(out=xo[b][:], in0=acc[:], in1=xt[b][:], op=ALU.add),
                       reads=[t_acc, t_xt[b]], writes=[t_xo[b]])
                    if last:
                        op("act", lambda e: e.activation(out=acc[:], in_=xo[b][:], func=AF.Square, accum_out=ss[:, 0:1]),
                           reads=[t_xo[b]], writes=[t_acc, t_ss])
                        op("dve", lambda e: e.tensor_scalar(out=ss[:, 1:2], in0=ss[:, 0:1], scalar1=1.0 / 1024.0, scalar2=EPS,
                                                            op0=ALU.mult, op1=ALU.add), reads=[t_ss], writes=[t_ss])
                        op("pool", lambda e: e.tensor_tensor(out=ss[:, 2:3], in0=ss[:, 1:2], in1=cm05[:, 0:1], op=ALU.pow), reads=[t_ss, t_c], writes=[t_ss])
                        op("dve", lambda e: e.scalar_tensor_tensor(out=xo[b][:], in0=xo[b][:], scalar=ss[:, 2:3], in1=gf[:],
                                                                   op0=ALU.mult, op1=ALU.mult), reads=[t_xo[b], t_ss, t_gf], writes=[t_xo[b]])
                    if i < 16:
                        dma("sp", x_new[i * 128:(i + 1) * 128, :], xo[b][:], reads=[t_xo[b]], writes=[t_xnew], multi=(i > 0))
                    else:
                        dma("sp", xc_new[(i - 16) * 128:(i - 15) * 128, :], xo[b][:], reads=[t_xo[b]], writes=[t_xcnew], multi=(i > 16))
                fw.barrier()
        print("build_layer", l, "ninst", fw.ninst, "nsem", fw.nsem, {k: v.cnt for k, v in fw.E.items()})
    except _Stop:
        print("build_layer stopped at", stop)
    return nc


def build_fused(NCOL, lam_inits):
    nc = bass.Bass("TRN2", target_bir_lowering=False, num_devices=8)
    IOA = bass.IndirectOffsetOnAxis
    x1 = nc.dram_tensor("x1_int", [2048, 1024], F32, kind="Internal").ap()
    xc1 = nc.dram_tensor("xc1_int", [256, 1024], F32, kind="Internal").ap()
    G1 = nc.dram_tensor("G1_int", [4096, 1024], F32, kind="Internal").ap()
    xsh = nc.dram_tensor("xsh", [8 * 2048, 1024], F32, kind="Internal", addr_space="Shared").ap()
    sidx_d = nc.dram_tensor("sidx", [128, 16], I32, kind="ExternalInput").ap()
    gidx_d = nc.dram_tensor("gidx", [128, 32], I32, kind="ExternalInput").ap()
    semstack = ExitStack()
    fw = FW(nc, semstack, "_g")
    build_layer(0, NCOL, lam_inits[0], nc=nc, io={"x_new": x1, "xc_new": xc1, "fw": fw}, sfx="_0")
    fw.recycle()
    with ExitStack() as st:
        sidx = st.enter_context(nc.sbuf_tensor("sidx_sb", [128, 16], I32)); t_si = Tk("sidx")
        gidx = st.enter_context(nc.sbuf_tensor("gidx_sb", [128, 32], I32)); t_gi = Tk("gidx")
        tl = [st.enter_context(nc.sbuf_tensor("xch%d" % i, [128, 1024], F32)) for i in range(4)]
        t_tl = [Tk("xch%d" % i) for i in range(4)]
        t_sh = Tk("xsh"); t_g1 = Tk("G1")
        fw.dma("sp", sidx[:], sidx_d, writes=[t_si])
        fw.dma("sp", gidx[:], gidx_d, writes=[t_gi])
        for i in range(16):
            b = i % 4
            fw.dma("sp", tl[b][:], x1[i * 128:(i + 1) * 128, :], writes=[t_tl[b]])
            fw.idma(xsh, IOA(ap=sidx[:, i:i + 1], axis=0), tl[b][:], None, reads=[t_si, t_tl[b]], writes=[t_sh], multi=(i > 0))
        fw.barrier()
        nc.all_core_barrier()
        for t in range(32):
            b = t % 4
            fw.idma(tl[b][:], None, xsh, IOA(ap=gidx[:, t:t + 1], axis=0), reads=[t_gi], writes=[t_tl[b]])
            fw.dma("sp", G1[t * 128:(t + 1) * 128, :], tl[b][:], reads=[t_tl[b]], writes=[t_g1], multi=(t > 0))
        fw.recycle()
    out = nc.dram_tensor("out", [2048, 1024], F32, kind="ExternalOutput").ap()
    build_layer(1, NCOL, lam_inits[1], nc=nc, io={"x_own": x1, "G": G1, "xc": xc1, "x_new": out, "xc_new": xc1, "pre_barrier": True, "fw": fw}, sfx="_1")
    semstack.close()
    return nc


_PROG = {}
_HOSTC = {}


def _get_prog(l, dbg=False, stop=None):
    key = (l, dbg, stop)
    if key not in _PROG:
        _PROG[key] = build_layer(l, NCOL, LAM_INIT[l], dbg=dbg, stop=stop)
    return _PROG[key]


def _layer_maps(inp, l, x_own_l, G_l, xc_l):
    f32 = np.float32
    cols = w_all_cols()
    W_all = np.ascontiguousarray(inp["w_in"][l][:, cols])
    lamv = np.concatenate([inp["lam_q1"][l], inp["lam_k1"][l], inp["lam_q2"][l], inp["lam_k2"][l]]).reshape(1, 128).astype(f32)
    g_sub2 = np.concatenate([inp["g_sub"][l], inp["g_sub"][l]]).reshape(128, 1).astype(f32)
    w_pool = np.zeros((64, 4, 128), f32)
    for g in range(4):
        w_pool[:, g, (g % 2) * 64:(g % 2) * 64 + 64] = inp["w_pool"][l][g]
    s_pool = np.ascontiguousarray(inp["s_pool"][l].reshape(2, 128).T)
    par_tabs = {}
    for par in range(2):
        wsT, bfull = sgu_tables(inp["w_sgu"][l], inp["b_sgu"][l], par)
        par_tabs[par] = dict(
            wsT=wsT, bfull=bfull,
            ntab=np.ascontiguousarray(nbr_tables(inp["rpb"][l], par).reshape(10, 64, 4 * 9 * 128)),
            ropeK=rope_table(G_IDX, CTX), ropeQ=rope_table(own_token_idx(par), CTX),
            pband=pool_band_tables(par))
    shared = dict(
        w_mod=inp["w_mod"][l], b_mod=inp["b_mod"][l].reshape(1, 6144), g_mix=inp["g_mix"][l].reshape(1, 1024),
        g_ffn=inp["g_ffn"][l].reshape(1, 1024), g_fin=inp["g_final"].reshape(1, 1024), W_all=W_all, w_out=inp["w_out"][l],
        lamv=lamv, g_sub=g_sub2, w_pool=w_pool, s_pool=s_pool, g_sgu=inp["g_sgu"][l].reshape(1, 256),
        w_router=inp["w_router"][l], b_router=inp["b_router"][l].reshape(1, 32),
        w_gu=np.ascontiguousarray(inp["w_gu"][l].reshape(32, 8, 128, 2048).transpose(0, 2, 1, 3)).reshape(32 * 128, 8 * 2048),
        b_gu=inp["b_gu"][l],
        w_down=np.ascontiguousarray(inp["w_down"][l].reshape(32, 8, 128, 1024).transpose(0, 2, 1, 3)).reshape(32 * 128, 8 * 1024),
        b_down=inp["b_down"][l])
    maps = []
    for core in range(8):
        b, par = core // 2, core % 2
        cT = np.zeros((128, 16), f32)
        cT[:, 0::2] = inp["c"][b].reshape(8, 128).T
        cT[:, 1::2] = inp["c_ctx"].reshape(8, 128).T
        m = dict(shared)
        m.update(par_tabs[par])
        m.update(x_own=x_own_l[core], G=G_l[b], xc=xc_l[core], cT=cT)
        maps.append({k: np.ascontiguousarray(v, dtype=f32) for k, v in m.items()})
    return maps


_SHARED_IN = ("ropeK", "ropeQ", "pband", "cT", "g_fin")
_FUSED = [True]


def kernel(_dbg=False, _layers=(0, 1), _stop=None, **inputs):
    inp = {k: np.asarray(v) for k, v in inputs.items()}
    x = inp["x"]
    own = [own_token_idx(0), own_token_idx(1)]
    x_own_l = [np.ascontiguousarray(x[c // 2][own[c % 2]]) for c in range(8)]
    G_l = [np.ascontiguousarray(x[b][G_IDX]) for b in range(4)]
    xc_l = [inp["ctx"][c // 2] for c in range(8)]
    if _FUSED[0] and not _dbg:
        if "fused" not in _PROG:
            _PROG["fused"] = build_fused(NCOL, LAM_INIT)
        m0 = _layer_maps(inp, 0, x_own_l, G_l, xc_l)
        m1 = _layer_maps(inp, 1, x_own_l, G_l, xc_l)
        maps = []
        for c in range(8):
            d = {}
            for k, v in m0[c].items():
                d[k if k in _SHARED_IN else k + "_0"] = v
            for k, v in m1[c].items():
                if k in _SHARED_IN or k in ("x_own", "G", "xc"):
                    continue
                d[k + "_1"] = v
            ar = np.arange(128, dtype=np.int64)[:, None]
            d["sidx"] = (c * 2048 + np.arange(16)[None, :] * 128 + ar).astype(np.int32)
            d["gidx"] = ((c // 2) * 4096 + np.arange(32)[None, :] * 128 + ar).astype(np.int32)
            maps.append(d)
        res = run_bass_kernel_spmd(_PROG["fused"], maps, core_ids=list(range(8))).results
        out = np.zeros((4, 4096, 1024), np.float32)
        for c in range(8):
            out[c // 2][own[c % 2]] = np.asarray(res[c]["out"])
        return out
    res = None
    for l in _layers:
        maps = _layer_maps(inp, l, x_own_l, G_l, xc_l)
        if _stop is not None and _stop.partition("#")[0] in ("mod", "proj", "pool", "nbr", "nbrA", "nbrB", "nbrC", "kv", "diff", "xmid", "route"):
            maps = [{k: v for k, v in m.items() if k not in ("w_gu", "b_gu", "w_down", "b_down")} for m in maps[:2]]
            return run_bass_kernel_spmd(_get_prog(l, _dbg, _stop), maps, core_ids=[0, 1]).results
        res = run_bass_kernel_spmd(_get_prog(l, _dbg, _stop), maps, core_ids=list(range(8))).results
        if _dbg:
            return res
        x_own_l = [np.asarray(res[c]["x_new"]) for c in range(8)]
        G_l = [np.concatenate([x_own_l[2 * b], x_own_l[2 * b + 1]], axis=0) for b in range(4)]
        if l == 0:
            xc_l = [np.asarray(res[c]["xc_new"]) for c in range(8)]
    out = np.zeros((4, 4096, 1024), np.float32)
    for c in range(8):
        out[c // 2][own[c % 2]] = x_own_l[c]
    return out
```
